# Optimizing a Trainium2 kernel written in Bass

```python
import jax, jax.numpy as jnp
from jax import lax
import numpy as np

D_MODEL = 1024
BATCH = 8
SEQ = 4096
DEPTH = 4

MIX_WIDTH = D_MODEL // 2
N_BRANCH = 3
A_WIDTH = MIX_WIDTH
A_BLOCKS = 8
A_BLOCK = A_WIDTH // A_BLOCKS
A_CONV = 4
A_C = 8.0
B_HEADS = 4
B_DK = MIX_WIDTH // B_HEADS
B_DV = MIX_WIDTH // B_HEADS
B_WIDTH = B_HEADS * B_DV
B_CONV = 4
B_CHUNK = 64
C_HEADS = 4
C_DV = MIX_WIDTH // C_HEADS
C_DK = C_DV // 2
C_WIDTH = C_HEADS * C_DV
C_RANK = 16
C_TAU = 16.0
C_CHUNK = 64
FFN_HIDDEN = -(-(8 * D_MODEL) // (3 * 256)) * 256
IN_SPLITS = (
    A_WIDTH, A_WIDTH,
    3 * B_HEADS * B_DK, B_WIDTH, B_HEADS, B_HEADS,
    C_HEADS * C_DK, C_HEADS * C_DK, C_WIDTH, C_RANK, C_WIDTH,
    N_BRANCH * D_MODEL,
)
IN_WIDTH = sum(IN_SPLITS)

kernel_name = "hybrid_rglru_gdn_gla_deepnorm"


def _layer_norm(x, g, b, eps=1e-5):
    xf = x.astype(jnp.float32)
    mu = jnp.mean(xf, -1, keepdims=True)
    var = jnp.mean(jnp.square(xf - mu), -1, keepdims=True)
    return ((xf - mu) * lax.rsqrt(var + eps) * g + b).astype(x.dtype)


def _rms_norm(x, w, eps=1e-6):
    xf = x.astype(jnp.float32)
    return xf * lax.rsqrt(jnp.mean(xf * xf, -1, keepdims=True) + eps) * w


def _l2norm(x, eps=1e-6):
    return x * lax.rsqrt(jnp.sum(x * x, -1, keepdims=True) + eps)


def _causal_dwconv(x, w):
    K = w.shape[0]
    S = x.shape[1]
    xp = jnp.pad(x, ((0, 0), (K - 1, 0), (0, 0)))
    return sum(xp[:, k:k + S] * w[k] for k in range(K))


def _rg_lru(x, w_r, b_r, w_i, b_i, lam):
    Bsz, S, W = x.shape
    xb = x.reshape(Bsz, S, A_BLOCKS, A_BLOCK)
    r = jax.nn.sigmoid(jnp.einsum("bshi,hij->bshj", xb, w_r).reshape(Bsz, S, W) + b_r)
    i = jax.nn.sigmoid(jnp.einsum("bshi,hij->bshj", xb, w_i).reshape(Bsz, S, W) + b_i)
    log_a = -A_C * r * jax.nn.softplus(-lam)
    a = jnp.exp(log_a)
    u = jnp.sqrt(-jnp.expm1(2.0 * log_a)) * (i * x)

    def combine(left, right):
        a1, b1 = left
        a2, b2 = right
        return a1 * a2, a2 * b1 + b2

    _, h = lax.associative_scan(combine, (a, u), axis=1)
    return h


def _gated_delta_rule(q, k, v, g, beta):
    Bsz, S, H, DK = q.shape
    DV = v.shape[-1]
    C = B_CHUNK
    N = S // C
    q = q.reshape(Bsz, N, C, H, DK) * (DK ** -0.5)
    k = k.reshape(Bsz, N, C, H, DK)
    v = v.reshape(Bsz, N, C, H, DV)
    beta = beta.reshape(Bsz, N, C, H)
    g = jnp.cumsum(g.reshape(Bsz, N, C, H), axis=2)
    causal = jnp.tril(jnp.ones((C, C), bool))[None, None, None]
    strict = jnp.tril(jnp.ones((C, C), bool), -1)[None, None, None]
    gh = g.transpose(0, 1, 3, 2)
    diff = gh[..., :, None] - gh[..., None, :]
    decay = jnp.where(causal, jnp.exp(jnp.where(causal, diff, 0.0)), 0.0)
    kk = jnp.einsum("bnihd,bnjhd->bnhij", k, k)
    A = jnp.where(strict, beta.transpose(0, 1, 3, 2)[..., :, None] * kk * decay, 0.0)
    eye = jnp.eye(C, dtype=A.dtype)
    rhs = jnp.concatenate([v * beta[..., None], k * (beta * jnp.exp(g))[..., None]], -1)
    rhs = rhs.transpose(0, 1, 3, 2, 4)
    sol = lax.linalg.triangular_solve(A + eye, rhs, left_side=True, lower=True,
                                      unit_diagonal=True)
    u = sol[..., :DV]
    w = sol[..., DV:]
    attn = jnp.einsum("bnihd,bnjhd->bnhij", q, k) * decay
    q_g = (q * jnp.exp(g)[..., None]).transpose(1, 0, 3, 2, 4)
    k_dec = (k * jnp.exp(g[:, :, -1:, :] - g)[..., None]).transpose(1, 0, 3, 2, 4)
    d_last = jnp.exp(g[:, :, -1, :]).transpose(1, 0, 2)
    xs = (u.transpose(1, 0, 2, 3, 4), w.transpose(1, 0, 2, 3, 4), q_g, k_dec,
          attn.transpose(1, 0, 2, 3, 4), d_last)

    def step(state, inp):
        u_n, w_n, qg_n, kd_n, at_n, dl_n = inp
        v_new = u_n - jnp.einsum("bhck,bhkv->bhcv", w_n, state)
        o = jnp.einsum("bhck,bhkv->bhcv", qg_n, state) + jnp.einsum("bhij,bhjv->bhiv", at_n, v_new)
        state = state * dl_n[..., None, None] + jnp.einsum("bhck,bhcv->bhkv", kd_n, v_new)
        return state, o

    s0 = jnp.zeros((Bsz, H, DK, DV), q.dtype)
    _, o = lax.scan(step, s0, xs)
    return o.transpose(1, 0, 3, 2, 4).reshape(Bsz, S, H, DV)


def _gla(q, k, v, log_a):
    Bsz, S, H, DK = q.shape
    DV = v.shape[-1]
    C = C_CHUNK
    N = S // C
    q = q.reshape(Bsz, N, C, H, DK) * (DK ** -0.5)
    k = k.reshape(Bsz, N, C, H, DK)
    v = v.reshape(Bsz, N, C, H, DV)
    b = jnp.cumsum(log_a.reshape(Bsz, N, C, H, DK), axis=2)
    b_last = b[:, :, -1:]
    q_in = q * jnp.exp(b)
    k_in = k * jnp.exp(-b)
    k_dec = k * jnp.exp(b_last - b)
    causal = jnp.tril(jnp.ones((C, C), bool))[None, None, None]
    attn = jnp.where(causal, jnp.einsum("bnihd,bnjhd->bnhij", q_in, k_in), 0.0)
    o_intra = jnp.einsum("bnhij,bnjhv->bnihv", attn, v)
    xs = (q_in.transpose(1, 0, 2, 3, 4), k_dec.transpose(1, 0, 2, 3, 4),
          v.transpose(1, 0, 2, 3, 4), jnp.exp(b_last[:, :, 0]).transpose(1, 0, 2, 3))

    def step(state, inp):
        qi_n, kd_n, v_n, dl_n = inp
        o = jnp.einsum("bchk,bhkv->bchv", qi_n, state)
        state = state * dl_n[..., None] + jnp.einsum("bchk,bchv->bhkv", kd_n, v_n)
        return state, o

    s0 = jnp.zeros((Bsz, H, DK, DV), q.dtype)
    _, o_inter = lax.scan(step, s0, xs)
    o = o_intra + o_inter.transpose(1, 0, 2, 3, 4)
    return o.reshape(Bsz, S, H, DV)


def _mixer(x, w_in, a_conv_w, a_conv_b, a_w_r, a_b_r, a_w_i, a_b_i, a_lambda,
           b_conv_w, b_a_log, b_dt_bias, b_norm_w, c_w_g2, c_b_g2, c_norm_w,
           gate_b, w_branch, w_out):
    f32 = jnp.float32
    Bsz, S, D = x.shape
    proj = jnp.einsum("bsd,de->bse", x, w_in).astype(f32)
    offs = np.cumsum(IN_SPLITS)[:-1].tolist()
    (pa_x, pa_g, pb_qkv, pb_z, pb_beta, pb_alpha,
     pc_q, pc_k, pc_v, pc_g, pc_r, p_merge) = jnp.split(proj, offs, axis=-1)

    xa = _causal_dwconv(pa_x, a_conv_w) + a_conv_b
    ya = _rg_lru(xa, a_w_r, a_b_r, a_w_i, a_b_i, a_lambda) * jax.nn.gelu(pa_g)

    qkv = jax.nn.silu(_causal_dwconv(pb_qkv, b_conv_w))
    bq, bk, bv = jnp.split(qkv, 3, axis=-1)
    bq = _l2norm(bq.reshape(Bsz, S, B_HEADS, B_DK))
    bk = _l2norm(bk.reshape(Bsz, S, B_HEADS, B_DK))
    bv = bv.reshape(Bsz, S, B_HEADS, B_DV)
    beta = jax.nn.sigmoid(pb_beta)
    g = -jnp.exp(b_a_log.astype(f32)) * jax.nn.softplus(pb_alpha + b_dt_bias)
    ob = _gated_delta_rule(bq, bk, bv, g, beta)
    yb = (_rms_norm(ob, b_norm_w) * jax.nn.silu(pb_z.reshape(Bsz, S, B_HEADS, B_DV))
          ).reshape(Bsz, S, B_WIDTH)

    log_a = jax.nn.log_sigmoid(jnp.einsum("bsr,rk->bsk", pc_g, c_w_g2) + c_b_g2) / C_TAU
    oc = _gla(pc_q.reshape(Bsz, S, C_HEADS, C_DK), pc_k.reshape(Bsz, S, C_HEADS, C_DK),
              pc_v.reshape(Bsz, S, C_HEADS, C_DV), log_a.reshape(Bsz, S, C_HEADS, C_DK))
    yc = (_rms_norm(oc, c_norm_w) * jax.nn.silu(pc_r.reshape(Bsz, S, C_HEADS, C_DV))
          ).reshape(Bsz, S, C_WIDTH)

    gates = jax.nn.sigmoid(p_merge + gate_b).reshape(Bsz, S, N_BRANCH, D)
    merged = sum(gates[:, :, gi] * jnp.einsum("bsc,cd->bsd", y, w_branch[gi])
                 for gi, y in enumerate((ya, yb, yc)))
    return jnp.einsum("bsd,de->bse", merged, w_out).astype(x.dtype)


def _swiglu(x, w1, w3, w2):
    h = jax.nn.silu(jnp.einsum("bsd,df->bsf", x, w1)) * jnp.einsum("bsd,df->bsf", x, w3)
    return jnp.einsum("bsf,fd->bsd", h, w2).astype(x.dtype)


def setup_inputs(seed: int = 0) -> dict:
    key = jax.random.key(seed)
    ks = iter(jax.random.split(key, 32))
    L, D = DEPTH, D_MODEL
    out_scale = (8.0 * DEPTH) ** -0.25

    def nrm(shape, scale):
        return jax.random.normal(next(ks), shape, jnp.float32) * scale

    x = nrm((BATCH, SEQ, D), 1.0)
    w_in = nrm((L, D, IN_WIDTH), D ** -0.5)
    a_conv_w = nrm((L, A_CONV, A_WIDTH), A_CONV ** -0.5)
    a_conv_b = nrm((L, A_WIDTH), 0.02)
    a_w_r = nrm((L, A_BLOCKS, A_BLOCK, A_BLOCK), A_BLOCK ** -0.5)
    a_b_r = nrm((L, A_WIDTH), 0.02)
    a_w_i = nrm((L, A_BLOCKS, A_BLOCK, A_BLOCK), A_BLOCK ** -0.5)
    a_b_i = nrm((L, A_WIDTH), 0.02)
    a_pow = jax.random.uniform(next(ks), (L, A_WIDTH), jnp.float32, 0.9, 0.999)
    a_root = a_pow ** (1.0 / A_C)
    a_lambda = jnp.log(a_root) - jnp.log1p(-a_root)
    b_conv_w = nrm((L, B_CONV, 3 * B_HEADS * B_DK), B_CONV ** -0.5)
    b_a_log = jnp.log(jax.random.uniform(next(ks), (L, B_HEADS), jnp.float32, 1.0, 16.0))
    dt = jnp.exp(jax.random.uniform(next(ks), (L, B_HEADS), jnp.float32,
                                    np.log(1e-3), np.log(1e-1)))
    b_dt_bias = dt + jnp.log(-jnp.expm1(-dt))
    b_norm_w = 1.0 + nrm((L, B_DV), 0.02)
    c_w_g2 = nrm((L, C_RANK, C_HEADS * C_DK), C_RANK ** -0.5)
    c_b_g2 = nrm((L, C_HEADS * C_DK), 0.02)
    c_norm_w = 1.0 + nrm((L, C_DV), 0.02)
    gate_b = nrm((L, N_BRANCH * D), 0.02)
    w_branch = nrm((L, N_BRANCH, MIX_WIDTH, D), MIX_WIDTH ** -0.5)
    w_out = nrm((L, D, D), D ** -0.5 * out_scale)
    ln1_g = 1.0 + nrm((L, D), 0.02)
    ln1_b = nrm((L, D), 0.02)
    ffn_w1 = nrm((L, D, FFN_HIDDEN), D ** -0.5)
    ffn_w3 = nrm((L, D, FFN_HIDDEN), D ** -0.5)
    ffn_w2 = nrm((L, FFN_HIDDEN, D), FFN_HIDDEN ** -0.5 * out_scale)
    ln2_g = 1.0 + nrm((L, D), 0.02)
    ln2_b = nrm((L, D), 0.02)
    return {"x": x, "w_in": w_in, "a_conv_w": a_conv_w, "a_conv_b": a_conv_b,
            "a_w_r": a_w_r, "a_b_r": a_b_r, "a_w_i": a_w_i, "a_b_i": a_b_i,
            "a_lambda": a_lambda, "b_conv_w": b_conv_w, "b_a_log": b_a_log,
            "b_dt_bias": b_dt_bias, "b_norm_w": b_norm_w, "c_w_g2": c_w_g2,
            "c_b_g2": c_b_g2, "c_norm_w": c_norm_w, "gate_b": gate_b,
            "w_branch": w_branch, "w_out": w_out, "ln1_g": ln1_g, "ln1_b": ln1_b,
            "ffn_w1": ffn_w1, "ffn_w3": ffn_w3, "ffn_w2": ffn_w2,
            "ln2_g": ln2_g, "ln2_b": ln2_b}


def reference(x, w_in, a_conv_w, a_conv_b, a_w_r, a_b_r, a_w_i, a_b_i, a_lambda,
              b_conv_w, b_a_log, b_dt_bias, b_norm_w, c_w_g2, c_b_g2, c_norm_w,
              gate_b, w_branch, w_out, ln1_g, ln1_b, ffn_w1, ffn_w3, ffn_w2,
              ln2_g, ln2_b):
    alpha = (2.0 * DEPTH) ** 0.25
    for l in range(DEPTH):
        mix = _mixer(x, w_in[l], a_conv_w[l], a_conv_b[l], a_w_r[l], a_b_r[l], a_w_i[l],
                     a_b_i[l], a_lambda[l], b_conv_w[l], b_a_log[l], b_dt_bias[l],
                     b_norm_w[l], c_w_g2[l], c_b_g2[l], c_norm_w[l], gate_b[l],
                     w_branch[l], w_out[l])
        x = _layer_norm(alpha * x + mix, ln1_g[l], ln1_b[l])
        ffn = _swiglu(x, ffn_w1[l], ffn_w3[l], ffn_w2[l])
        x = _layer_norm(alpha * x + ffn, ln2_g[l], ln2_b[l])
    return x
```

```python
import numpy as np
from contextlib import ExitStack
import concourse.bass as bass
import concourse.mybir as mybir
from concourse.alu_op_type import AluOpType as ALU
from concourse.bass_utils import run_bass_kernel_spmd

F32 = mybir.dt.float32
BF16 = mybir.dt.bfloat16
AF = mybir.ActivationFunctionType

D = 1024
KC = 8
T = 512
NCH = 4
DEPTH = 4
SEQ = 4096
FF = 2816
NFB = 22
NSLOT = 8
ALPHA = (2.0 * DEPTH) ** 0.25

O_AX, O_AG, O_BQ, O_BK, O_BV, O_BZ = 0, 512, 1024, 1536, 2048, 2560
O_BETA, O_ALPHA, O_CQ, O_CK, O_CV, O_CG, O_CR, O_MG = 3072, 3076, 3080, 3336, 3592, 4104, 4120, 4632

PP_ACW, PP_ACB, PP_ABR, PP_ABI, PP_ALAM, PP_BCW, PP_BNW, PP_CNW, PP_CBG, PP_GB = 0, 16, 20, 24, 28, 32, 80, 81, 82, 84
PP_LN1G, PP_LN1B, PP_LN2G, PP_LN2B = 108, 116, 124, 132
NPP = 140


class _Op:
    __slots__ = ("eng", "fn", "deps", "signal", "count", "dma_sem", "dma_val", "idx")


class Sched:
    ENGS = ("pe", "act", "dve", "pool", "sp")

    def __init__(self, nc):
        self.nc = nc
        self.ops = []
        self.last_w = {}
        self.readers = {}
        self.regions = {}

    def region(self, name, space, lo, hi):
        self.regions[name] = (space, lo, hi)

    def _ov(self, r):
        reg = self.regions.get(r)
        if reg is None:
            return (r,)
        sp, lo, hi = reg
        return [n for n, (s2, l2, h2) in self.regions.items() if s2 == sp and l2 < hi and lo < h2]

    def op(self, eng, fn, reads=(), writes=(), dma=None):
        o = _Op()
        o.eng = eng; o.fn = fn; o.signal = False; o.count = 0
        o.dma_sem = dma; o.dma_val = 0; o.idx = len(self.ops)
        deps = {}
        for r0 in reads:
            for r in self._ov(r0):
                w = self.last_w.get(r)
                if w is not None:
                    deps[w.idx] = (w, "raw")
            if r0[:2] in ("pf", "pb"):
                for rd in self.readers.get(r0, ()):
                    if rd.eng != eng and rd.idx not in deps:
                        deps[rd.idx] = (rd, "raw")
        for w0 in writes:
            for wr in self._ov(w0):
                w = self.last_w.get(wr)
                if w is not None:
                    deps.setdefault(w.idx, (w, "waw"))
                for rd in self.readers.get(wr, ()):
                    if rd.idx not in deps:
                        deps[rd.idx] = (rd, "war")
        o.deps = []
        for (d, kind) in deps.values():
            if d.dma_sem is None and dma is None and d.eng == eng:
                if eng == "pe":
                    continue
                if kind == "war":
                    continue
            o.deps.append(d)
        for r in reads:
            self.readers.setdefault(r, []).append(o)
        for w0 in writes:
            for wr in self._ov(w0):
                self.last_w[wr] = o
                self.readers[wr] = []
        self.ops.append(o)
        return o

    def emit(self, sems, dma_sems):
        nc = self.nc
        for o in self.ops:
            for d in o.deps:
                if d.dma_sem is None:
                    d.signal = True
        cnt = {e: 0 for e in self.ENGS}
        dcnt = {k: 0 for k in dma_sems}
        for o in self.ops:
            if o.dma_sem is not None:
                dcnt[o.dma_sem] += 16
                o.dma_val = dcnt[o.dma_sem]
            elif o.signal:
                cnt[o.eng] += 1
                o.count = cnt[o.eng]
        final_dma = dict(dcnt)
        per_eng = {e: [o for o in self.ops if o.eng == e] for e in self.ENGS}
        with nc.Block() as block:
            def body(ename):
                def f(eng):
                    waited = {}
                    for o in per_eng[ename]:
                        need = {}
                        for d in o.deps:
                            if d.dma_sem is not None:
                                key = ("d", d.dma_sem); val = d.dma_val
                            else:
                                key = ("e", d.eng); val = d.count
                            if val > need.get(key, 0):
                                need[key] = val
                        for key, val in need.items():
                            if waited.get(key, 0) >= val:
                                continue
                            waited[key] = val
                            h = dma_sems[key[1]] if key[0] == "d" else sems[key[1]]
                            eng.wait_ge(h, val)
                        ins = o.fn(eng)
                        if o.dma_sem is not None:
                            ins.then_inc(dma_sems[o.dma_sem], 16)
                        elif o.signal:
                            ins.then_inc(sems[ename], 1)
                    if ename == "sp":
                        for k, v in final_dma.items():
                            if v > 0:
                                eng.wait_ge(dma_sems[k], v)
                return f
            block.tensor(body("pe"))
            block.scalar(body("act"))
            block.vector(body("dve"))
            block.gpsimd(body("pool"))
            block.sync(body("sp"))


def unit_list():
    u = []
    for b in range(4):
        u.append(("in", O_AX + 128 * b, 8))
        if b == 0:
            u.append(("awri", 0, 8))
        u.append(("in", O_AG + 128 * b, 8))
    u.append(("small", 0, 8))
    u.append(("cwg", 0, 2))
    for b in range(4): u.append(("in", O_BQ + 128 * b, 8))
    for b in range(4): u.append(("in", O_BK + 128 * b, 8))
    for b in range(4): u.append(("in", O_BV + 128 * b, 8))
    for b in range(4): u.append(("in", O_BZ + 128 * b, 8))
    for b in range(2): u.append(("in", O_CQ + 128 * b, 8))
    for b in range(2): u.append(("in", O_CK + 128 * b, 8))
    for b in range(4): u.append(("in", O_CV + 128 * b, 8))
    for b in range(4): u.append(("in", O_CR + 128 * b, 8))
    for j in range(8):
        for gi in range(3): u.append(("in", O_MG + 1024 * gi + 128 * j, 8))
        u.append(("br01", j, 8))
        u.append(("br2", j, 4))
    for j in range(8): u.append(("wout", j, 8))
    for i in range(NFB):
        u.append(("w1", i, 8))
        u.append(("w3", i, 8))
    for j in range(8):
        u.append(("w2", (j, 0), 8))
        u.append(("w2", (j, 8), 8))
        u.append(("w2", (j, 16), 6))
    return u


UNITS = unit_list()
NU = len(UNITS)


def host_wstream(inp, l):
    w_in = inp["w_in"][l]
    out = np.zeros((NU, 128, 8, 128), np.float32)

    def kmajor(mat):
        nk = mat.shape[0] // 128
        return mat.reshape(nk, 128, 128).transpose(1, 0, 2)

    for ui, (kind, arg, nk) in enumerate(UNITS):
        if kind == "in":
            out[ui] = kmajor(w_in[:, arg:arg + 128])
        elif kind == "small":
            blk = np.zeros((1024, 128), np.float32)
            blk[:, 0:16] = w_in[:, O_CG:O_CG + 16]
            blk[:, 16:20] = w_in[:, O_BETA:O_BETA + 4]
            blk[:, 20:24] = w_in[:, O_ALPHA:O_ALPHA + 4]
            out[ui] = kmajor(blk)
        elif kind == "awri":
            for wi, name in enumerate(("a_w_r", "a_w_i")):
                w = inp[name][l]
                for b in range(4):
                    m = np.zeros((128, 128), np.float32)
                    m[0:64, 0:64] = w[2 * b]
                    m[64:128, 64:128] = w[2 * b + 1]
                    out[ui, :, wi * 4 + b, :] = m
        elif kind == "cwg":
            w = inp["c_w_g2"][l]
            out[ui, 0:16, 0, :] = w[:, 0:128]
            out[ui, 0:16, 1, :] = w[:, 128:256]
        elif kind == "br01":
            j = arg
            wb = inp["w_branch"][l]
            out[ui, :, 0:4, :] = kmajor(wb[0][:, 128 * j:128 * (j + 1)])
            out[ui, :, 4:8, :] = kmajor(wb[1][:, 128 * j:128 * (j + 1)])
        elif kind == "br2":
            j = arg
            wb = inp["w_branch"][l]
            out[ui, :, 0:4, :] = kmajor(wb[2][:, 128 * j:128 * (j + 1)])
        elif kind == "wout":
            out[ui] = kmajor(inp["w_out"][l][:, 128 * arg:128 * (arg + 1)])
        elif kind == "w1":
            out[ui] = kmajor(inp["ffn_w1"][l][:, 128 * arg:128 * (arg + 1)])
        elif kind == "w3":
            out[ui] = kmajor(inp["ffn_w3"][l][:, 128 * arg:128 * (arg + 1)])
        elif kind == "w2":
            j, k0 = arg
            out[ui, :, 0:nk, :] = kmajor(inp["ffn_w2"][l][128 * k0:128 * (k0 + nk), 128 * j:128 * (j + 1)])
    return out


def host_consts():
    j = np.arange(128)[:, None]
    i = np.arange(128)[None, :]
    cst = np.zeros((128, 6, 128), np.float32)
    cst[:, 0] = (i == j)
    cst[:, 1] = (i >= j)
    cst[:, 2] = np.where(i < j, -1.0e4, 0.0)
    cst[:, 3] = (i > j)
    cst[:, 4] = 1.0
    cst[:, 5] = 1.0 / 1024.0
    esel = np.zeros((24, 4, 128), np.float32)
    for h in range(4):
        esel[16 + h, h, :] = 1.0
    return cst, esel


def host_params(inp, depth):
    pp = np.zeros((128, depth, NPP), np.float32)

    def fm(v):
        return v.reshape(-1, 128).T

    for l in range(depth):
        pp[:, l, PP_ACW:PP_ACW + 16] = inp["a_conv_w"][l].reshape(4, 4, 128).transpose(2, 1, 0).reshape(128, 16)
        pp[:, l, PP_ACB:PP_ACB + 4] = fm(inp["a_conv_b"][l])
        pp[:, l, PP_ABR:PP_ABR + 4] = fm(inp["a_b_r"][l])
        pp[:, l, PP_ABI:PP_ABI + 4] = fm(inp["a_b_i"][l])
        pp[:, l, PP_ALAM:PP_ALAM + 4] = fm(inp["a_lambda"][l])
        pp[:, l, PP_BCW:PP_BCW + 48] = inp["b_conv_w"][l].reshape(4, 12, 128).transpose(2, 1, 0).reshape(128, 48)
        pp[:, l, PP_BNW] = inp["b_norm_w"][l]
        pp[:, l, PP_CNW] = inp["c_norm_w"][l]
        pp[:, l, PP_CBG:PP_CBG + 2] = fm(inp["c_b_g2"][l])
        pp[:, l, PP_GB:PP_GB + 24] = fm(inp["gate_b"][l])
        pp[:, l, PP_LN1G:PP_LN1G + 8] = fm(inp["ln1_g"][l])
        pp[:, l, PP_LN1B:PP_LN1B + 8] = fm(inp["ln1_b"][l])
        pp[:, l, PP_LN2G:PP_LN2G + 8] = fm(inp["ln2_g"][l])
        pp[:, l, PP_LN2B:PP_LN2B + 8] = fm(inp["ln2_b"][l])
    pt = np.zeros((1, depth * 8), np.float32)
    for l in range(depth):
        pt[0, l * 8:l * 8 + 4] = inp["b_a_log"][l]
        pt[0, l * 8 + 4:l * 8 + 8] = inp["b_dt_bias"][l]
    return pp, pt


class _Stop(Exception):
    pass


def build_nc(seq=SEQ, depth=DEPTH, stop=None):
    NG = seq // T
    alpha2 = 2.0 * (2.0 * depth) ** 0.25
    nc = bass.Bass("TRN2", target_bir_lowering=False)
    xT = nc.dram_tensor("xT", [KC, 128, seq], F32, kind="ExternalInput").ap()
    wst = nc.dram_tensor("wst", [depth, NU, 128, 8, 128], F32, kind="ExternalInput").ap()
    cstd = nc.dram_tensor("cst", [128, 6, 128], F32, kind="ExternalInput").ap()
    eseld = nc.dram_tensor("esel", [24, 4, 128], F32, kind="ExternalInput").ap()
    ppd = nc.dram_tensor("pp", [128, depth, NPP], F32, kind="ExternalInput").ap()
    ptd = nc.dram_tensor("pt", [1, depth * 8], F32, kind="ExternalInput").ap()
    outd = nc.dram_tensor("out", [KC, 128, seq], F32, kind="ExternalOutput").ap()
    dbg_out = {}

    with ExitStack() as es:
        def sb(name, shape, dt=F32):
            return es.enter_context(nc.sbuf_tensor(name, shape, dt))

        def psum(name, shape, dt=F32):
            return es.enter_context(nc.psum_tensor(name, shape, dt))

        S = Sched(nc)
        xres = sb("xres", [128, KC, T])
        xb = sb("xb", [128, KC, T], BF16)
        wring = sb("wring", [128, NSLOT, 8, 128], BF16)
        cst = sb("cstf", [128, 6, 128])
        identf, Uf, NEGM, Lstrict, onesf, ones1k = (cst[:, k, :] for k in range(6))
        identb = sb("identb", [128, 128], BF16)
        onesb = sb("onesb", [128, 128], BF16)
        ones1kb = sb("ones1kb", [128, 128], BF16)
        esel = sb("eself", [24, 4, 128])
        pp = sb("ppf", [128, depth, NPP])
        pder = sb("pder", [128, depth, 48])
        ptb = sb("ptb", [128, depth * 8])
        nexpal = sb("nexpal", [128, depth * 8])
        cb = sb("cbias", [128, 4])
        hst = sb("hst", [128, depth, 4])
        tailA = sb("tailA", [128, depth, 4, 3])
        tailB = sb("tailB", [128, depth, 12, 3])
        SB = sb("SBst", [128, depth, 4, 128])
        SBb = sb("SBstb", [128, depth, 4, 128], BF16)
        SC = sb("SCst", [128, depth, 2, 128])
        SCb = sb("SCstb", [128, depth, 2, 128], BF16)
        NP1 = 3
        P1 = sb("P1", [128, NP1, T + 3])
        NTMP = 8
        tmp = sb("tmp", [128, NTMP, T])
        awrib = sb("awrib", [128, 8, 128], BF16)
        cwgb = sb("cwgb", [128, 2, 128], BF16)
        sqb2 = sb("sqb2", [128, 2, T], BF16)
        t44 = sb("t44", [128, NCH, 4]); t44b = sb("t44b", [128, NCH, 4])
        xa = sb("xa", [128, T])
        xab = sb("xab", [128, T], BF16)
        qn = sb("qn", [128, 4, T], BF16)
        kn = sb("kn", [128, 4, T], BF16)
        vT2 = sb("vT2", [128, 4, T], BF16)
        betabc = sb("betabc", [128, 4, T], BF16)
        smallfm = sb("smallfm", [24, T])
        pcgb = sb("pcgb", [16, T], BF16)
        tok8 = sb("tok8", [128, NCH, 8])
        hbeta = sb("hbeta", [128, NCH, 4])
        beta = sb("beta", [128, NCH, 4])
        gtok = sb("gtok", [128, NCH, 4])
        lC = sb("lC", [128, 2, T])
        qC = sb("qC", [128, 2, T])
        kC = sb("kC", [128, 2, T])
        vCT = sb("vCT", [128, 4, T], BF16)
        gr = sb("gr", [128, 4, T], BF16)
        scr = sb("scr", [128, 12288], BF16)
        ya = scr[:, 0:2048].rearrange("p (b t) -> p b t", b=4)
        yb = scr[:, 2048:4096].rearrange("p (b t) -> p b t", b=4)
        yc = scr[:, 4096:6144].rearrange("p (b t) -> p b t", b=4)
        merged = scr[:, 6144:10240].rearrange("p (b t) -> p b t", b=8)
        gz = scr[:, 10240:12288].rearrange("p (b t) -> p b t", b=4)
        hact = scr[:, 0:NFB * T].rearrange("p (b t) -> p b t", b=NFB)
        for b in range(4):
            S.region("ya%d" % b, "scr", b * T, (b + 1) * T)
            S.region("yb%d" % b, "scr", 2048 + b * T, 2048 + (b + 1) * T)
            S.region("yc%d" % b, "scr", 4096 + b * T, 4096 + (b + 1) * T)
            S.region("gz%d" % b, "scr", 10240 + b * T, 10240 + (b + 1) * T)
        for b in range(8):
            S.region("mg%d" % b, "scr", 6144 + b * T, 6144 + (b + 1) * T)
        for b in range(NFB):
            S.region("h%d" % b, "scr", b * T, (b + 1) * T)
        gc4 = sb("gc4", [128, 4]); ngc4 = sb("ngc4", [128, 4]); eg4 = sb("eg4", [128, 4])
        egl4 = sb("egl4", [128, 4]); dl4 = sb("dl4", [128, 4]); bge4 = sb("bge4", [128, 4])
        t4 = sb("t4", [128, 4])
        gbc = sb("gbc", [128, 4, 128])
        egbc = sb("egbc", [128, 4, 128])
        Dm = sb("Dm", [128, 4, 128])
        Dsb = sb("Dsb", [128, 4, 128])
        vb_t = sb("vb_t", [128, 4, 128], BF16); kbg_t = sb("kbg_t", [128, 4, 128], BF16); kd_t = sb("kd_t", [128, 4, 128], BF16)
        nAT = sb("nAT", [128, 4, 128]); nA = sb("nA", [128, 4, 128]); TTb = sb("TTb", [128, 4, 128], BF16); attnT = sb("attnT", [128, 4, 128], BF16)
        QP = sb("QP", [128, 4, 2, 256]); PB = sb("PBm", [128, 4, 2, 128])
        nwT = sb("nwT", [128, 4, 128], BF16); vnew = sb("vnew", [128, 4, 128], BF16); qgT = sb("qgT", [128, 4, 128], BF16)
        on_t = sb("on_t", [128, 8, 128], BF16); junk = sb("junk", [128, 8, 128], BF16)
        ssq1 = sb("ssq1", [128, 8]); ln1 = sb("ln1", [128, 8]); rs1 = sb("rs1", [128, 8])
        clC = sb("clC", [128, 2, 128]); ebC = sb("ebC", [128, 2, 128]); enbC = sb("enbC", [128, 2, 128]); edecC = sb("edecC", [128, 2, 128])
        nclC = sb("nclC", [128, 2]); dlcC = sb("dlcC", [128, 2])
        qin = sb("qin", [128, 2, 128], BF16); kin = sb("kin", [128, 2, 128], BF16); kdecT = sb("kdecT", [128, 2, 128], BF16)
        kdtok = sb("kdtok", [128, 2, 128], BF16); vtokC = sb("vtokC", [128, 4, 128], BF16); attnC = sb("attnC", [128, 4, 128], BF16)
        msq = gbc[:].rearrange("p h i -> p (h i)")
        rstd = egbc[:].rearrange("p h i -> p (h i)")
        meansb = Dm[:].rearrange("p h i -> p (h i)")

        pf = [psum("pf%d" % i, [128, 512]) for i in range(8)]
        PFN = ["pf%d" % i for i in range(8)]

        sems = {e: es.enter_context(nc.semaphore("s_" + e)) for e in Sched.ENGS}
        dnames = ["w%d" % i for i in range(NSLOT)] + ["ld", "st", "c0", "c1", "c2", "c3"]
        dsems = {k: es.enter_context(nc.semaphore("d_" + k)) for k in dnames}

        CBI = {1.0: 0, 1e-6: 1, 4e-5: 2, 0.0: 3}

        def ACT(out, in_, func, r, w, scale=None, bias=None, accum=None):
            kw = {}
            if scale is not None: kw["scale"] = scale
            if bias is None:
                bias = 0.0
            if isinstance(bias, float):
                k_ = CBI[bias]
                bias = cb[0:in_.shape[0], k_:k_ + 1]
                r = list(r) + ["cb"]
            kw["bias"] = bias
            if accum is not None: kw["accum_out"] = accum
            S.op("act", lambda e: e.activation(out=out, in_=in_, func=func, **kw), reads=r, writes=w)

        def TS(out, in0, s1, s2, op0, op1, r, w, eng="dve"):
            if op1 is None:
                S.op(eng, lambda e: e.tensor_scalar(out=out, in0=in0, scalar1=s1, scalar2=None, op0=op0), reads=r, writes=w)
            else:
                S.op(eng, lambda e: e.tensor_scalar(out=out, in0=in0, scalar1=s1, scalar2=s2, op0=op0, op1=op1), reads=r, writes=w)

        def STT(out, in0, sc, in1, op0, op1, r, w, eng="dve"):
            S.op(eng, lambda e: e.scalar_tensor_tensor(out=out, in0=in0, scalar=sc, in1=in1, op0=op0, op1=op1), reads=r, writes=w)

        def TT(out, in0, in1, op, r, w, eng="dve"):
            S.op(eng, lambda e: e.tensor_tensor(out=out, in0=in0, in1=in1, op=op), reads=r, writes=w)

        def CP(out, in_, r, w, eng="dve"):
            if eng == "act":
                ACT(out, in_, AF.Identity, r, w)
            else:
                S.op(eng, lambda e: e.tensor_copy(out=out, in_=in_), reads=r, writes=w)

        def MM(out, lhsT, rhs, start, stop, r, w):
            S.op("pe", lambda e: e.matmul(out, lhsT=lhsT, rhs=rhs, start=start, stop=stop), reads=r, writes=w)

        def TR(out, in_, ident, r, w):
            S.op("pe", lambda e: e.transpose(out, in_, ident), reads=r, writes=w)

        glist = []
        for g in range(NG):
            for l in range(depth):
                for ui, (kind, arg, nk) in enumerate(UNITS):
                    glist.append((l, ui, nk))
        wstate = {"issued": 0, "next": 0}

        def w_issue():
            n = wstate["issued"]
            if n >= len(glist):
                return
            l, ui, nk = glist[n]
            slot = n % NSLOT
            S.op("pool", lambda e: e.dma_start(out=wring[:, slot, 0:nk, :], in_=wst[l, ui, :, 0:nk, :]),
                 writes=["ws%d" % slot], dma="w%d" % slot)
            wstate["issued"] += 1

        def w_get(kind):
            n = wstate["next"]
            l, ui, nk = glist[n]
            assert UNITS[ui][0] == kind, (UNITS[ui], kind)
            slot = n % NSLOT
            wstate["next"] += 1
            return wring[:, slot], "ws%d" % slot

        def w_done():
            w_issue()

        for _ in range(NSLOT):
            w_issue()
        S.op("sp", lambda e: e.dma_start(out=cst[:], in_=cstd), writes=["cst"], dma="c0")
        S.op("sp", lambda e: e.dma_start(out=esel[:], in_=eseld), writes=["esel"], dma="c1")
        S.op("sp", lambda e: e.dma_start(out=pp[:], in_=ppd), writes=["pp"], dma="c2")
        S.op("sp", lambda e: e.dma_start(out=ptb[:], in_=ptd.partition_broadcast(128)), writes=["ptb"], dma="c3")
        for v_, k_ in CBI.items():
            S.op("dve", lambda e, v_=v_, k_=k_: e.memset(cb[:, k_:k_ + 1], v_), writes=["cb"])
        CP(identb[:], identf, ["cst"], ["identb"])
        CP(onesb[:], onesf, ["cst"], ["onesb"])
        CP(ones1kb[:], ones1k, ["cst"], ["ones1kb"])
        S.op("dve", lambda e: e.memset(hst[:], 0.0), writes=["hst"])
        S.op("dve", lambda e: e.memset(tailA[:], 0.0), writes=["tailA"])
        S.op("dve", lambda e: e.memset(tailB[:], 0.0), writes=["tailB"])
        S.op("dve", lambda e: e.memset(SB[:], 0.0), writes=["SB"])
        S.op("dve", lambda e: e.memset(SBb[:], 0.0), writes=["SBb"])
        S.op("dve", lambda e: e.memset(SC[:], 0.0), writes=["SC"])
        S.op("dve", lambda e: e.memset(SCb[:], 0.0), writes=["SCb"])
        for l in range(depth):
            TS(pder[:, l, 0:8], pp[:, l, PP_ABR:PP_ABR + 8], 0.5, None, ALU.mult, None, ["pp"], ["pder"])
            ACT(pder[:, l, 8:12], pp[:, l, PP_ALAM:PP_ALAM + 4], AF.Exp, ["pp", "pder"], ["pder"], scale=-1.0)
            ACT(pder[:, l, 8:12], pder[:, l, 8:12], AF.Ln, ["pder"], ["pder"], bias=1.0)
            TS(pder[:, l, 12:16], pder[:, l, 8:12], -8.0, None, ALU.mult, None, ["pder"], ["pder"])
            TS(pder[:, l, 8:12], pder[:, l, 8:12], -4.0, None, ALU.mult, None, ["pder"], ["pder"])
            TS(pder[:, l, 16:18], pp[:, l, PP_BNW:PP_BNW + 2], 0.5, None, ALU.mult, None, ["pp", "pder"], ["pder"])
            TS(pder[:, l, 18:20], pp[:, l, PP_CBG:PP_CBG + 2], -1.0, None, ALU.mult, None, ["pp", "pder"], ["pder"])
            TS(pder[:, l, 20:44], pp[:, l, PP_GB:PP_GB + 24], 0.5, None, ALU.mult, None, ["pp", "pder"], ["pder"])
        ACT(nexpal[:], ptb[:], AF.Exp, ["ptb"], ["nexpal"])
        TS(nexpal[:], nexpal[:], -1.0, None, ALU.mult, None, ["nexpal"], ["nexpal"])

        bank_rr = {"i": 0}

        def newbank():
            i = (0, 1, 2, 3, 5, 6)[bank_rr["i"] % 6]
            bank_rr["i"] += 1
            return pf[i], PFN[i]

        tmp_rr = {"i": 0}

        def newtmp():
            i = tmp_rr["i"] % NTMP
            tmp_rr["i"] += 1
            return tmp[:, i, :], "tmp%d" % i

        p1_rr = {"i": 0}

        def newp1():
            i = p1_rr["i"] % NP1
            p1_rr["i"] += 1
            return P1[:, i, :], "P1_%d" % i

        def proj(kind="in"):
            wv, wn = w_get(kind)
            bk, bn = newbank()
            for k in range(KC):
                MM(bk[:], wv[:, k, :], xb[:, k, :], k == 0, k == KC - 1, [wn, "xb"], [bn])
            w_done()
            return bk, bn

        def run_all():
          for g in range(NG):
              tsl = slice(g * T, (g + 1) * T)
              S.op("sp", lambda e, tsl=tsl: e.dma_start(out=xres[:], in_=xT[:, :, tsl].rearrange("k p t -> p k t")),
                   writes=["xres"], dma="ld")
              for k in range(KC):
                  CP(xb[:, k, :], xres[:, k, :], ["xres"], ["xb"], eng=("act" if k % 2 else "dve"))
              for l in range(depth):
                  pd = pder[:, l, :]
                  ppl = pp[:, l, :]

                  awrib_n = "awrib"
                  for b in range(4):
                      bx, bxn = proj()
                      if b == 0:
                          wv, wn = w_get("awri")
                          CP(awrib[:], wv[:], [wn], [awrib_n], eng="act")
                          w_done()
                      p1, p1n = newp1()
                      CP(p1[:, 0:3], tailA[:, l, b, :], ["tailA"], [p1n])
                      CP(p1[:, 3:T + 3], bx[:], [bxn], [p1n], eng="act")
                      cw = ppl[:, PP_ACW + 4 * b:PP_ACW + 4 * b + 4]
                      TS(xa[:], p1[:, 3:T + 3], cw[:, 3:4], ppl[:, PP_ACB + b:PP_ACB + b + 1], ALU.mult, ALU.add, [p1n, "pp"], ["xa"])
                      for kk in range(3):
                          STT(xa[:], p1[:, kk:kk + T], cw[:, kk:kk + 1], xa[:], ALU.mult, ALU.add, [p1n, "pp", "xa"], ["xa"])
                      CP(tailA[:, l, b, :], p1[:, T:T + 3], [p1n], ["tailA"])
                      CP(xab[:], xa[:], ["xa"], ["xab"], eng="act")
                      zr, zrn = newbank()
                      MM(zr[:], awrib[:, b, :], xab[:], True, True, [awrib_n, "xab"], [zrn])
                      zi, zin = newbank()
                      MM(zi[:], awrib[:, 4 + b, :], xab[:], True, True, [awrib_n, "xab"], [zin])
                      tr_, trn = newtmp()
                      ACT(tr_, zr[:], AF.Tanh, [zrn, "pder"], [trn], scale=0.5, bias=pd[:, b:b + 1])
                      ti_, tin = newtmp()
                      ACT(ti_, zi[:], AF.Tanh, [zin, "pder"], [tin], scale=0.5, bias=pd[:, 4 + b:5 + b])
                      a_, an = newtmp()
                      TS(a_, tr_, pd[:, 8 + b:9 + b], pd[:, 8 + b:9 + b], ALU.mult, ALU.add, [trn, "pder"], [an])
                      e2_, e2n = newtmp()
                      ACT(e2_, a_, AF.Exp, [an], [e2n], scale=2.0)
                      ACT(a_, a_, AF.Exp, [an], [an])
                      TS(e2_, e2_, -1.0, 1.0, ALU.mult, ALU.add, [e2n], [e2n])
                      ACT(e2_, e2_, AF.Ln, [e2n], [e2n])
                      ACT(e2_, e2_, AF.Exp, [e2n], [e2n], scale=0.5)
                      STT(ti_, ti_, 1.0, xa[:], ALU.add, ALU.mult, [tin, "xa"], [tin])
                      TT(ti_, ti_, e2_, ALU.mult, [tin, e2n], [tin])
                      S.op("dve", lambda e, o=tr_, a_=a_, u=ti_, l=l, b=b: e.tensor_tensor_scan(
                          out=o, data0=a_, data1=u, initial=hst[:, l, b:b + 1], op0=ALU.mult, op1=ALU.add),
                          reads=[an, tin, "hst"], writes=[trn])
                      CP(hst[:, l, b:b + 1], tr_[:, T - 1:T], [trn], ["hst"])
                      bg, bgn = proj()
                      g2, g2n = e2_, e2n
                      ACT(g2, bg[:], AF.Square, [bgn], [g2n])
                      TS(g2, g2, 0.044715, 1.0, ALU.mult, ALU.add, [g2n], [g2n])
                      TT(g2, g2, bg[:], ALU.mult, [g2n, bgn], [g2n])
                      ACT(g2, g2, AF.Tanh, [g2n], [g2n], scale=0.7978845608028654)
                      STT(g2, g2, 1.0, bg[:], ALU.add, ALU.mult, [g2n, bgn], [g2n])
                      STT(ya[:, b, :], tr_, 0.25, g2, ALU.mult, ALU.mult, [trn, g2n], ["ya%d" % b])

                  if stop == "A":
                      raise _Stop()
                  wv, wn = w_get("small")
                  bk, bn = newbank()
                  for k in range(KC):
                      MM(bk[0:24, :], wv[:, k, 0:24], xb[:, k, :], k == 0, k == KC - 1, [wn, "xb"], [bn])
                  for c in range(NCH):
                      for k in range(KC):
                          MM(pf[4][:, c * 8:(c + 1) * 8], xb[:, k, c * 128:(c + 1) * 128], wv[:, k, 16:24], k == 0, k == KC - 1,
                             [wn, "xb"], [PFN[4]])
                  w_done()
                  CP(smallfm[:], bk[0:24, :], [bn], ["smallfm"], eng="act")
                  CP(pcgb[:], bk[0:16, :], [bn], ["pcgb"])
                  CP(tok8[:], pf[4][:, 0:32].rearrange("p (c e) -> p c e", c=NCH), [PFN[4]], ["tok8"], eng="act")
                  ACT(t44[:], tok8[:, :, 0:4], AF.Tanh, ["tok8"], ["t44"], scale=0.5)
                  TS(beta[:], t44[:], 0.5, 0.5, ALU.mult, ALU.add, ["t44"], ["beta"])
                  TS(hbeta[:], t44[:], 0.25, 0.25, ALU.mult, ALU.add, ["t44"], ["hbeta"])
                  TT(t44b[:], tok8[:, :, 4:8], ptb[:, l * 8 + 4:l * 8 + 8].unsqueeze(1).to_broadcast([128, NCH, 4]), ALU.add,
                     ["tok8", "ptb"], ["t44b"])
                  ACT(t44b[:], t44b[:], AF.Exp, ["t44b"], ["t44b"])
                  ACT(t44b[:], t44b[:], AF.Ln, ["t44b"], ["t44b"], bias=1.0)
                  TT(gtok[:], t44b[:], nexpal[:, l * 8:l * 8 + 4].unsqueeze(1).to_broadcast([128, NCH, 4]), ALU.mult,
                     ["t44b", "nexpal"], ["gtok"])
                  for h in range(4):
                      bb, bbn = newbank()
                      MM(bb[:], esel[:, h, :], smallfm[:], True, True, ["esel", "smallfm"], [bbn])
                      t_, tn = newtmp()
                      ACT(t_, bb[:], AF.Tanh, [bbn], [tn], scale=0.5)
                      TS(betabc[:, h, :], t_, 0.5, 0.5, ALU.mult, ALU.add, [tn], ["betabc%d" % h])
                  wv, wn = w_get("cwg")
                  CP(cwgb[:], wv[:, 0:2, :], [wn], ["cwgb"], eng="act")
                  w_done()
                  for b in range(2):
                      bb, bbn = newbank()
                      MM(bb[:], cwgb[0:16, b, :], pcgb[:], True, True, ["cwgb", "pcgb"], [bbn])
                      t_, tn = newtmp()
                      ACT(t_, bb[:], AF.Exp, [bbn, "pder"], [tn], scale=-1.0, bias=pd[:, 18 + b:19 + b])
                      ACT(lC[:, b, :], t_, AF.Ln, [tn], ["lC%d" % b], bias=1.0)

                  if stop == "small":
                      raise _Stop()
                  def run_pipe(gen_list, W=2):
                      active = []
                      it = iter(gen_list)
                      while True:
                          while len(active) < W:
                              try:
                                  active.append(next(it))
                              except StopIteration:
                                  break
                          if not active:
                              break
                          for g_ in list(active):
                              try:
                                  next(g_)
                              except StopIteration:
                                  active.remove(g_)

                  def gen_B(ci):
                      h = ci % 4
                      sq_ = sqb2[:, ci % 2, :]; sqn_ = "sqb%d" % (ci % 2)
                      bk, bn = proj()
                      p1, p1n = newp1()
                      CP(p1[:, 0:3], tailB[:, l, ci, :], ["tailB%d" % ci], [p1n])
                      CP(p1[:, 3:T + 3], bk[:], [bn], [p1n], eng="act")
                      yield
                      cw = ppl[:, PP_BCW + 4 * ci:PP_BCW + 4 * ci + 4]
                      c_, cn = newtmp()
                      TS(c_, p1[:, 3:T + 3], cw[:, 3:4], None, ALU.mult, None, [p1n, "pp"], [cn])
                      for kk in range(3):
                          STT(c_, p1[:, kk:kk + T], cw[:, kk:kk + 1], c_, ALU.mult, ALU.add, [p1n, "pp", cn], [cn])
                      CP(tailB[:, l, ci, :], p1[:, T:T + 3], [p1n], ["tailB%d" % ci])
                      yield
                      t_, tn = newtmp()
                      ACT(t_, c_, AF.Tanh, [cn], [tn], scale=0.5)
                      yield
                      if ci >= 8:
                          STT(vT2[:, h, :], t_, 1.0, c_, ALU.add, ALU.mult, [tn, cn], ["vT2_%d" % h])
                          return
                      STT(c_, t_, 1.0, c_, ALU.add, ALU.mult, [tn, cn], [cn])
                      yield
                      ACT(sq_, c_, AF.Square, [cn], [sqn_])
                      yield
                      b2, b2n = newbank()
                      MM(b2[:], onesb[:], sq_, True, True, ["onesb", sqn_], [b2n])
                      yield
                      ACT(t_, b2[:], AF.Ln, [b2n], [tn], scale=0.25, bias=1e-6)
                      ACT(t_, t_, AF.Exp, [tn], [tn], scale=-0.5)
                      yield
                      if ci < 4:
                          STT(qn[:, h, :], c_, 0.5 * (128.0 ** -0.5), t_, ALU.mult, ALU.mult, [cn, tn], ["qn%d" % h])
                      else:
                          STT(kn[:, h, :], c_, 0.5, t_, ALU.mult, ALU.mult, [cn, tn], ["kn%d" % h])

                  run_pipe([gen_B(ci) for ci in range(12)], W=2)
                  for h in range(4):
                      bk, bn = proj()
                      t_, tn = newtmp()
                      ACT(t_, bk[:], AF.Tanh, [bn], [tn], scale=0.5)
                      STT(t_, t_, 1.0, bk[:], ALU.add, ALU.mult, [tn, bn], [tn])
                      TS(gz[:, h, :], t_, pd[:, 16:17], None, ALU.mult, None, [tn, "pder"], ["gz%d" % h])
                  if stop == "B":
                      raise _Stop()
                  for b in range(2):
                      bk, bn = proj()
                      CP(qC[:, b, :], bk[:], [bn], ["qC%d" % b], eng="act")
                  for b in range(2):
                      bk, bn = proj()
                      CP(kC[:, b, :], bk[:], [bn], ["kC%d" % b], eng="act")
                  for h in range(4):
                      bk, bn = proj()
                      CP(vCT[:, h, :], bk[:], [bn], ["vCT%d" % h], eng="act")
                  for h in range(4):
                      bk, bn = proj()
                      t_, tn = newtmp()
                      ACT(t_, bk[:], AF.Tanh, [bn], [tn], scale=0.5)
                      STT(t_, t_, 1.0, bk[:], ALU.add, ALU.mult, [tn, bn], [tn])
                      TS(gr[:, h, :], t_, pd[:, 17:18], None, ALU.mult, None, [tn, "pder"], ["gr%d" % h])

                  if stop == "C":
                      raise _Stop()
                  pbv = pf[7][:].bitcast(BF16)
                  PB7 = PFN[7]

                  def epilogue_g(u, o_ap, o_bn, dst, gate, rn, wn_, trcol, pbv=None, PB7=None):
                      if pbv is None:
                          pbv = pf[7][:].bitcast(BF16); PB7 = PFN[7]
                      U_ = "%d" % u
                      ACT(junk[:, u, :], o_ap, AF.Square, [o_bn], ["junk" + U_, "ssq" + U_], accum=ssq1[:, u:u + 1])
                      yield
                      ACT(ln1[:, u:u + 1], ssq1[:, u:u + 1], AF.Ln, ["ssq" + U_], ["ln" + U_], scale=1.0 / 128.0, bias=1e-6)
                      ACT(rs1[:, u:u + 1], ln1[:, u:u + 1], AF.Exp, ["ln" + U_], ["rs" + U_], scale=-0.5)
                      yield
                      TS(on_t[:, u, :], o_ap, rs1[:, u:u + 1], None, ALU.mult, None, [o_bn, "rs" + U_], ["on" + U_])
                      yield
                      TR(pbv[:, trcol:trcol + 128], on_t[:, u, :], identb[:], ["on" + U_, "identb"], [PB7])
                      yield
                      TT(dst, pbv[:, trcol:trcol + 128], gate, ALU.mult, [PB7, rn], [wn_])

                  def gdn_head(c, csl, h):
                      bank = pf[h]; bn = PFN[h]
                      knc = kn[:, h, csl]; qnc = qn[:, h, csl]
                      H = "%d" % h
                      tc0 = h * 256
                      sbh = SBb[:, l, h, :]; sbn = "SBb%d_%d" % (l, h); sfn = "SB%d_%d" % (l, h)
                      TR(pbv[:, tc0:tc0 + 128], knc, identb[:], ["kn" + H, "identb"], [PB7])
                      TR(pbv[:, tc0 + 128:tc0 + 256], vT2[:, h, csl], identb[:], ["vT2_" + H, "identb"], [PB7])
                      MM(bank[:, 0:128], knc, knc, True, True, ["kn" + H], [bn])
                      MM(bank[:, 128:256], knc, qnc, True, True, ["kn" + H, "qn" + H], [bn])
                      yield
                      TS(vb_t[:, h, :], pbv[:, tc0 + 128:tc0 + 256], hbeta[:, c, h:h + 1], None, ALU.mult, None, [PB7, "hbeta"], ["vb" + H])
                      TS(kbg_t[:, h, :], pbv[:, tc0:tc0 + 128], bge4[:, h:h + 1], None, ALU.mult, None, [PB7, "bge4"], ["kbg" + H])
                      TS(kd_t[:, h, :], pbv[:, tc0:tc0 + 128], egl4[:, h:h + 1], None, ALU.mult, None, [PB7, "egl4"], ["kd" + H])
                      STT(nAT[:, h, :], bank[:, 0:128], -1.0, Dsb[:, h, :], ALU.mult, ALU.mult, [bn, "Dsb"], ["nAT" + H])
                      TT(attnT[:, h, :], bank[:, 128:256], Dm[:, h, :], ALU.mult, [bn, "Dm"], ["attnT" + H])
                      TT(qgT[:, h, :], qnc, egbc[:, h, :], ALU.mult, ["qn" + H, "egbc"], ["qgT" + H])
                      yield
                      TR(bank[:, 256:384], nAT[:, h, :], identf, ["nAT" + H, "cst"], [bn])
                      yield
                      CP(nA[:, h, :], bank[:, 256:384], [bn], ["nA" + H], eng="act")
                      TT(QP[:, h, 1, 0:128], nAT[:, h, :], identf, ALU.add, ["nAT" + H, "cst"], ["QP1q" + H])
                      yield
                      MM(bank[:, 0:128], nA[:, h, :], nAT[:, h, :], True, True, ["nA" + H, "nAT" + H], [bn])
                      MM(bank[:, 128:256], nAT[:, h, :], nA[:, h, :], True, True, ["nA" + H, "nAT" + H], [bn])
                      yield
                      CP(QP[:, h, 1, 128:256], bank[:, 0:128], [bn], ["QP1p" + H], eng="act")
                      CP(PB[:, h, 1, :], bank[:, 128:256], [bn], ["PB1" + H], eng="act")
                      yield
                      for k in range(1, 7):
                          cur = k % 2; nxt = (k + 1) % 2
                          qc, pc_, pbc = "QP%dq%s" % (cur, H), "QP%dp%s" % (cur, H), "PB%d%s" % (cur, H)
                          qx, px, pbx = "QP%dq%s" % (nxt, H), "QP%dp%s" % (nxt, H), "PB%d%s" % (nxt, H)
                          if k < 6:
                              MM(bank[:, 0:256], PB[:, h, cur, :], QP[:, h, cur, :], True, True, [pbc, qc, pc_], [bn])
                              MM(bank[:, 256:384], QP[:, h, cur, 128:256], PB[:, h, cur, :], True, True, [pbc, pc_], [bn])
                              yield
                              TT(QP[:, h, nxt, 0:128], bank[:, 0:128], QP[:, h, cur, 0:128], ALU.add, [bn, qc], [qx])
                              CP(QP[:, h, nxt, 128:256], bank[:, 128:256], [bn], [px])
                              CP(PB[:, h, nxt, :], bank[:, 256:384], [bn], [pbx])
                              yield
                          else:
                              MM(bank[:, 0:128], PB[:, h, cur, :], QP[:, h, cur, 0:128], True, True, [pbc, qc], [bn])
                              yield
                              TT(TTb[:, h, :], bank[:, 0:128], QP[:, h, cur, 0:128], ALU.add, [bn, qc], ["TTb" + H])
                              yield
                      MM(bank[:, 0:128], kbg_t[:, h, :], TTb[:, h, :], True, True, ["kbg" + H, "TTb" + H], [bn])
                      yield
                      ACT(nwT[:, h, :], bank[:, 0:128], AF.Identity, [bn], ["nwT" + H], scale=-1.0)
                      yield
                      MM(bank[:, 128:256], TTb[:, h, :], vb_t[:, h, :], True, False, ["TTb" + H, "vb" + H], [bn])
                      MM(bank[:, 128:256], nwT[:, h, :], sbh, False, True, ["nwT" + H, sbn], [bn])
                      yield
                      CP(vnew[:, h, :], bank[:, 128:256], [bn], ["vnew" + H])
                      yield
                      MM(bank[:, 256:384], qgT[:, h, :], sbh, True, False, ["qgT" + H, sbn], [bn])
                      MM(bank[:, 256:384], attnT[:, h, :], vnew[:, h, :], False, True, ["attnT" + H, "vnew" + H], [bn])
                      MM(bank[:, 384:512], kd_t[:, h, :], vnew[:, h, :], True, True, ["kd" + H, "vnew" + H], [bn])
                      yield
                      STT(SB[:, l, h, :], SB[:, l, h, :], dl4[:, h:h + 1], bank[:, 384:512], ALU.mult, ALU.add, [sfn, "dl4", bn], [sfn])
                      yield
                      CP(sbh, SB[:, l, h, :], [sfn], [sbn], eng="act")
                      yield from epilogue_g(h, bank[:, 256:384], bn, yb[:, h, csl], gz[:, h, csl], "gz" + H, "yb" + H, tc0)

                  def gla_head(c, csl, h):
                      b = h // 2; po = 64 * (h % 2); psl = slice(po, po + 64)
                      bi = (4, 5, 6, 4)[h]; bank = pf[bi]; bn = PFN[bi]
                      H = "%d" % h
                      scn = "SC%d_%d" % (l, h); scbn = "SCb%d_%d" % (l, h)
                      bvw = bank[:].bitcast(BF16)
                      TR(bvw[:, 768:896], vCT[:, h, csl], identb[:], ["vCT" + H, "identb"], [bn])
                      MM(bank[:, 0:128], kin[psl, b, :], qin[psl, b, :], True, True, ["kin%d" % b, "qin%d" % b], [bn])
                      yield
                      CP(vtokC[:, h, :], bvw[:, 768:896], [bn], ["vtokC" + H], eng="act")
                      TT(attnC[:, h, :], bank[:, 0:128], Uf, ALU.mult, [bn, "cst"], ["attnC" + H])
                      yield
                      MM(bank[:, 128:256], attnC[:, h, :], vtokC[:, h, :], True, False, ["attnC" + H, "vtokC" + H], [bn])
                      MM(bank[:, 128:256], qin[psl, b, :], SCb[psl, l, b, :], False, True, ["qin%d" % b, scbn], [bn])
                      MM(bank[:, 256:384], kdtok[:, b, :], vtokC[:, h, :], True, True, ["kdtok%d" % b, "vtokC" + H], [bn])
                      yield
                      STT(SC[psl, l, b, :], SC[psl, l, b, :], dlcC[psl, b:b + 1], bank[psl, 256:384], ALU.mult, ALU.add,
                          [scn, "dlcC%d" % b, bn], [scn])
                      yield
                      CP(SCb[psl, l, b, :], SC[psl, l, b, :], [scn], [scbn], eng="act")
                      yield from epilogue_g(4 + h, bank[:, 128:256], bn, yc[:, h, csl], gr[:, h, csl], "gr" + H, "yc" + H, 768, bvw, bn)

                  for c in range(NCH):
                      csl = slice(c * 128, (c + 1) * 128)
                      g1 = pf[5][:].rearrange("p (h i) -> p h i", h=4)
                      g2_ = pf[6][:].rearrange("p (h i) -> p h i", h=4)
                      MM(pf[4][:, 0:4], Uf, gtok[:, c, :], True, True, ["cst", "gtok"], [PFN[4]])
                      CP(gc4[:], pf[4][:, 0:4], [PFN[4]], ["gc4"], eng="act")
                      TS(ngc4[:], gc4[:], -1.0, None, ALU.mult, None, ["gc4"], ["ngc4"])
                      ACT(eg4[:], gc4[:], AF.Exp, ["gc4"], ["eg4"])
                      for h in range(4):
                          TS(gbc[:, h, :], onesf, gtok[:, c, h:h + 1], None, ALU.mult, None, ["cst", "gtok"], ["gbc%d" % h])
                      for h in range(4):
                          MM(g1[:, h, :], gbc[:, h, :], Uf, True, True, ["gbc%d" % h, "cst"], [PFN[5]])
                      for h in range(4):
                          MM(g2_[:, h, :], gbc[:, h, :], Uf, True, False, ["gbc%d" % h, "cst"], [PFN[6]])
                          MM(g2_[:, h, :], identf, NEGM, False, True, ["cst"], [PFN[6]])
                      for b in range(2):
                          S.op("dve", lambda e, b=b, csl=csl: e.tensor_tensor_scan(
                              out=clC[:, b, :], data0=onesf, data1=lC[:, b, csl], initial=0.0, op0=ALU.mult, op1=ALU.add),
                              reads=["cst", "lC%d" % b], writes=["clC%d" % b])
                          cn_ = "clC%d" % b
                          ACT(ebC[:, b, :], clC[:, b, :], AF.Exp, [cn_], ["ebC%d" % b], scale=-1.0 / 16)
                          ACT(enbC[:, b, :], clC[:, b, :], AF.Exp, [cn_], ["enbC%d" % b], scale=1.0 / 16)
                          TS(nclC[:, b:b + 1], clC[:, b, 127:128], -1.0 / 16, None, ALU.mult, None, [cn_], ["nclC%d" % b])
                          ACT(edecC[:, b, :], clC[:, b, :], AF.Exp, [cn_, "nclC%d" % b], ["edecC%d" % b], scale=1.0 / 16, bias=nclC[:, b:b + 1])
                          ACT(dlcC[:, b:b + 1], nclC[:, b:b + 1], AF.Exp, ["nclC%d" % b], ["dlcC%d" % b])
                          STT(qin[:, b, :], qC[:, b, csl], 0.125, ebC[:, b, :], ALU.mult, ALU.mult, ["qC%d" % b, "ebC%d" % b], ["qin%d" % b])
                          TT(kin[:, b, :], kC[:, b, csl], enbC[:, b, :], ALU.mult, ["kC%d" % b, "enbC%d" % b], ["kin%d" % b])
                          TT(kdecT[:, b, :], kC[:, b, csl], edecC[:, b, :], ALU.mult, ["kC%d" % b, "edecC%d" % b], ["kdecT%d" % b])
                      ACT(egbc[:], g1, AF.Exp, [PFN[5]], ["egbc"])
                      for h in range(4):
                          ACT(Dm[:, h, :], g2_[:, h, :], AF.Exp, [PFN[6], "ngc4"], ["Dm"], bias=ngc4[:, h:h + 1])
                      for h in range(4):
                          ACT(egl4[:, h:h + 1], g1[:, h, 127:128], AF.Exp, [PFN[5], "ngc4"], ["egl4"], bias=ngc4[:, h:h + 1])
                      CP(dl4[:], egbc[:, :, 127], ["egbc"], ["dl4"])
                      TT(bge4[:], beta[:, c, :], eg4[:], ALU.mult, ["beta", "eg4"], ["bge4"])
                      TT(Dsb[:], Dm[:], Lstrict.unsqueeze(1).to_broadcast([128, 4, 128]), ALU.mult, ["Dm", "cst"], ["Dsb"])
                      TT(Dsb[:], Dsb[:], betabc[:, :, csl], ALU.mult, ["Dsb"] + ["betabc%d" % h for h in range(4)], ["Dsb"])
                      for b in range(2):
                          TR(pbv[:, b * 128:(b + 1) * 128], kdecT[:, b, :], identb[:], ["kdecT%d" % b, "identb"], [PB7])
                      for b in range(2):
                          CP(kdtok[:, b, :], pbv[:, b * 128:(b + 1) * 128], [PB7], ["kdtok%d" % b], eng="act")
                      def chain(*gs):
                          for g_ in gs:
                              yield from g_
                      gens = [gdn_head(c, csl, h) for h in range(4)] + [chain(gla_head(c, csl, 0), gla_head(c, csl, 3)),
                                                                         gla_head(c, csl, 1), gla_head(c, csl, 2)]
                      while gens:
                          for gen in list(gens):
                              try:
                                  next(gen)
                              except StopIteration:
                                  gens.remove(gen)

                  if stop == "mix" and l == depth - 1 and g == NG - 1:
                      raise _Stop()

                  for j in range(8):
                      tg = []
                      for gi in range(3):
                          bk, bn = proj()
                          t_, tn = newtmp()
                          ACT(t_, bk[:], AF.Tanh, [bn, "pder"], [tn], scale=0.5, bias=pd[:, 20 + gi * 8 + j:21 + gi * 8 + j])
                          tg.append((t_, tn))
                      wv, wn = w_get("br01")
                      b0, b0n = newbank()
                      for k in range(4):
                          MM(b0[:], wv[:, k, :], ya[:, k, :], k == 0, k == 3, [wn, "ya%d" % k], [b0n])
                      b1, b1n = newbank()
                      for k in range(4):
                          MM(b1[:], wv[:, 4 + k, :], yb[:, k, :], k == 0, k == 3, [wn, "yb%d" % k], [b1n])
                      w_done()
                      wv, wn = w_get("br2")
                      b2, b2n = newbank()
                      for k in range(4):
                          MM(b2[:], wv[:, k, :], yc[:, k, :], k == 0, k == 3, [wn, "yc%d" % k], [b2n])
                      w_done()
                      m_, mn = newtmp()
                      STT(m_, tg[0][0], 1.0, b0[:], ALU.add, ALU.mult, [tg[0][1], b0n], [mn])
                      STT(tg[1][0], tg[1][0], 1.0, b1[:], ALU.add, ALU.mult, [tg[1][1], b1n], [tg[1][1]])
                      TT(m_, m_, tg[1][0], ALU.add, [mn, tg[1][1]], [mn])
                      STT(tg[2][0], tg[2][0], 1.0, b2[:], ALU.add, ALU.mult, [tg[2][1], b2n], [tg[2][1]])
                      TT(merged[:, j, :], m_, tg[2][0], ALU.add, [mn, tg[2][1]], ["mg%d" % j])
                  for j in range(8):
                      wv, wn = w_get("wout")
                      bk, bn = newbank()
                      for k in range(KC):
                          MM(bk[:], wv[:, k, :], merged[:, k, :], k == 0, k == KC - 1, [wn, "mg%d" % k], [bn])
                      w_done()
                      STT(xres[:, j, :], xres[:, j, :], alpha2, bk[:], ALU.mult, ALU.add, ["xres", bn], ["xres"])

                  def layer_norm(gcol, bcol):
                      GBCN = ["gbc%d" % h_ for h_ in range(4)]
                      for k in range(KC):
                          MM(pf[4][:], ones1k, xres[:, k, :], k == 0, k == KC - 1, ["cst", "xres"], [PFN[4]])
                      be, ben = newbank()
                      for k in range(KC):
                          sq = sqb2[:, k % 2, :]; sqn = "sqb%d" % (k % 2)
                          ACT(sq, xres[:, k, :], AF.Square, ["xres"], [sqn])
                          MM(be[:], ones1kb[:], sq, k == 0, k == KC - 1, ["ones1kb", sqn], [ben])
                      CP(meansb, pf[4][:], [PFN[4]], ["Dm"], eng="act")
                      ACT(msq, pf[4][:], AF.Square, [PFN[4]], GBCN)
                      TT(rstd, be[:], msq, ALU.subtract, [ben, *GBCN], ["egbc"])
                      ACT(rstd, rstd, AF.Ln, ["egbc"], ["egbc"], bias=4e-5)
                      ACT(rstd, rstd, AF.Exp, ["egbc"], ["egbc"], scale=-0.5)
                      for k in range(KC):
                          t_, tn = newtmp()
                          TT(t_, xres[:, k, :], meansb, ALU.subtract, ["xres", "Dm"], [tn])
                          TT(t_, t_, rstd, ALU.mult, [tn, "egbc"], [tn])
                          ACT(xres[:, k, :], t_, AF.Identity, [tn, "pp"], ["xres"], scale=ppl[:, gcol + k:gcol + k + 1],
                              bias=ppl[:, bcol + k:bcol + k + 1])
                          ACT(xb[:, k, :], t_, AF.Identity, [tn, "pp"], ["xb"], scale=ppl[:, gcol + k:gcol + k + 1],
                              bias=ppl[:, bcol + k:bcol + k + 1])

                  layer_norm(PP_LN1G, PP_LN1B)
                  for i in range(NFB):
                      b1, b1n = proj("w1")
                      b3, b3n = proj("w3")
                      t_, tn = newtmp()
                      ACT(t_, b1[:], AF.Tanh, [b1n], [tn], scale=0.5)
                      STT(t_, t_, 1.0, b1[:], ALU.add, ALU.mult, [tn, b1n], [tn])
                      TT(hact[:, i, :], t_, b3[:], ALU.mult, [tn, b3n], ["h%d" % i])
                  for j in range(8):
                      bk, bn = newbank()
                      for (k0, nk) in ((0, 8), (8, 8), (16, 6)):
                          wv, wn = w_get("w2")
                          for k in range(nk):
                              MM(bk[:], wv[:, k, :], hact[:, k0 + k, :], (k0 + k) == 0, (k0 + k) == NFB - 1, [wn, "h%d" % (k0 + k)], [bn])
                          w_done()
                      STT(xres[:, j, :], xres[:, j, :], alpha2, bk[:], ALU.mult, ALU.add, ["xres", bn], ["xres"])
                  layer_norm(PP_LN2G, PP_LN2B)
              S.op("sp", lambda e, tsl=tsl: e.dma_start(out=outd[:, :, tsl].rearrange("k p t -> p k t"), in_=xres[:]),
                   reads=["xres"], dma="st")

        try:
            run_all()
        except _Stop:
            pass
        dbg_out = {}
        if stop == "mix":
            for nm, src in (("d_ya", ya), ("d_yb", yb), ("d_yc", yc)):
                dt_ = nc.dram_tensor(nm, [128, 4, T], BF16, kind="ExternalOutput").ap()
                S.op("sp", lambda e, dt_=dt_, src=src: e.dma_start(out=dt_, in_=src), reads=["ya%d" % b for b in range(4)] + ["yb%d" % b for b in range(4)] + ["yc%d" % b for b in range(4)], dma="st")
        S.emit(sems, dsems)
    return nc


_CACHE = {}


def host_inputs(inp, depth=DEPTH, seq=SEQ):
    cst, esel = host_consts()
    pp, pt = host_params(inp, depth)
    wst = np.stack([host_wstream(inp, l) for l in range(depth)], 0)
    shared = {"wst": wst, "cst": cst, "esel": esel, "pp": pp, "pt": pt}
    x = np.asarray(inp["x"], np.float32)
    maps = []
    for b in range(x.shape[0]):
        xT = np.ascontiguousarray(x[b].T).reshape(KC, 128, seq)
        m = dict(shared)
        m["xT"] = xT
        maps.append(m)
    return maps


def kernel(**inputs):
    inp = {k: np.asarray(v) for k, v in inputs.items()}
    B = inp["x"].shape[0]
    maps = host_inputs(inp)
    if "nc" not in _CACHE:
        _CACHE["nc"] = build_nc()
    nc = _CACHE["nc"]
    res = run_bass_kernel_spmd(nc, maps, core_ids=list(range(B)))
    out = np.empty((B, SEQ, D), np.float32)
    for b in range(B):
        o = np.asarray(res.results[b]["out"]).reshape(D, SEQ)
        out[b] = o.T
    return out
```

```python
import numpy as np
from contextlib import ExitStack
import concourse.bass as bass
import concourse.mybir as mybir
from concourse.alu_op_type import AluOpType as ALU
from concourse.bass_utils import run_bass_kernel_spmd

F32 = mybir.dt.float32
BF16 = mybir.dt.bfloat16
AF = mybir.ActivationFunctionType

D = 1024
KC = 8
T = 512
NCH = 4
DEPTH = 4
SEQ = 4096
FF = 2816
NFB = 22
NSLOT = 8
ALPHA = (2.0 * DEPTH) ** 0.25

O_AX, O_AG, O_BQ, O_BK, O_BV, O_BZ = 0, 512, 1024, 1536, 2048, 2560
O_BETA, O_ALPHA, O_CQ, O_CK, O_CV, O_CG, O_CR, O_MG = 3072, 3076, 3080, 3336, 3592, 4104, 4120, 4632

PP_ACW, PP_ACB, PP_ABR, PP_ABI, PP_ALAM, PP_BCW, PP_BNW, PP_CNW, PP_CBG, PP_GB = 0, 16, 20, 24, 28, 32, 80, 81, 82, 84
PP_LN1G, PP_LN1B, PP_LN2G, PP_LN2B = 108, 116, 124, 132
NPP = 140


class _Op:
    __slots__ = ("eng", "fn", "deps", "signal", "count", "dma_sem", "dma_val", "idx")


class Sched:
    ENGS = ("pe", "act", "dve", "pool", "sp")

    def __init__(self, nc):
        self.nc = nc
        self.ops = []
        self.last_w = {}
        self.readers = {}
        self.regions = {}

    def region(self, name, space, lo, hi):
        self.regions[name] = (space, lo, hi)

    def _ov(self, r):
        reg = self.regions.get(r)
        if reg is None:
            return (r,)
        sp, lo, hi = reg
        return [n for n, (s2, l2, h2) in self.regions.items() if s2 == sp and l2 < hi and lo < h2]

    def op(self, eng, fn, reads=(), writes=(), dma=None):
        o = _Op()
        o.eng = eng; o.fn = fn; o.signal = False; o.count = 0
        o.dma_sem = dma; o.dma_val = 0; o.idx = len(self.ops)
        deps = {}
        for r0 in reads:
            for r in self._ov(r0):
                w = self.last_w.get(r)
                if w is not None:
                    deps[w.idx] = (w, "raw")
            if r0[:2] in ("pf", "pb"):
                for rd in self.readers.get(r0, ()):
                    if rd.eng != eng and rd.idx not in deps:
                        deps[rd.idx] = (rd, "raw")
        for w0 in writes:
            for wr in self._ov(w0):
                w = self.last_w.get(wr)
                if w is not None:
                    deps.setdefault(w.idx, (w, "waw"))
                for rd in self.readers.get(wr, ()):
                    if rd.idx not in deps:
                        deps[rd.idx] = (rd, "war")
        o.deps = []
        for (d, kind) in deps.values():
            if d.dma_sem is None and dma is None and d.eng == eng:
                if eng == "pe":
                    continue
                if kind == "war":
                    continue
            o.deps.append(d)
        for r in reads:
            self.readers.setdefault(r, []).append(o)
        for w0 in writes:
            for wr in self._ov(w0):
                self.last_w[wr] = o
                self.readers[wr] = []
        self.ops.append(o)
        return o

    def emit(self, sems, dma_sems):
        nc = self.nc
        for o in self.ops:
            for d in o.deps:
                if d.dma_sem is None:
                    d.signal = True
        cnt = {e: 0 for e in self.ENGS}
        dcnt = {k: 0 for k in dma_sems}
        for o in self.ops:
            if o.dma_sem is not None:
                dcnt[o.dma_sem] += 16
                o.dma_val = dcnt[o.dma_sem]
            elif o.signal:
                cnt[o.eng] += 1
                o.count = cnt[o.eng]
        final_dma = dict(dcnt)
        per_eng = {e: [o for o in self.ops if o.eng == e] for e in self.ENGS}
        with nc.Block() as block:
            def body(ename):
                def f(eng):
                    waited = {}
                    for o in per_eng[ename]:
                        need = {}
                        for d in o.deps:
                            if d.dma_sem is not None:
                                key = ("d", d.dma_sem); val = d.dma_val
                            else:
                                key = ("e", d.eng); val = d.count
                            if val > need.get(key, 0):
                                need[key] = val
                        for key, val in need.items():
                            if waited.get(key, 0) >= val:
                                continue
                            waited[key] = val
                            h = dma_sems[key[1]] if key[0] == "d" else sems[key[1]]
                            eng.wait_ge(h, val)
                        ins = o.fn(eng)
                        if o.dma_sem is not None:
                            ins.then_inc(dma_sems[o.dma_sem], 16)
                        elif o.signal:
                            ins.then_inc(sems[ename], 1)
                    if ename == "sp":
                        for k, v in final_dma.items():
                            if v > 0:
                                eng.wait_ge(dma_sems[k], v)
                return f
            block.tensor(body("pe"))
            block.scalar(body("act"))
            block.vector(body("dve"))
            block.gpsimd(body("pool"))
            block.sync(body("sp"))


def unit_list():
    u = []
    for b in range(4):
        u.append(("in", O_AX + 128 * b, 8))
        if b == 0:
            u.append(("awri", 0, 8))
        u.append(("in", O_AG + 128 * b, 8))
    u.append(("small", 0, 8))
    u.append(("cwg", 0, 2))
    for b in range(4): u.append(("in", O_BQ + 128 * b, 8))
    for b in range(4): u.append(("in", O_BK + 128 * b, 8))
    for b in range(4): u.append(("in", O_BV + 128 * b, 8))
    for b in range(4): u.append(("in", O_BZ + 128 * b, 8))
    for b in range(2): u.append(("in", O_CQ + 128 * b, 8))
    for b in range(2): u.append(("in", O_CK + 128 * b, 8))
    for b in range(4): u.append(("in", O_CV + 128 * b, 8))
    for b in range(4): u.append(("in", O_CR + 128 * b, 8))
    for j in range(8):
        for gi in range(3): u.append(("in", O_MG + 1024 * gi + 128 * j, 8))
        u.append(("br01", j, 8))
        u.append(("br2", j, 4))
    for j in range(8): u.append(("wout", j, 8))
    for i in range(NFB):
        u.append(("w1", i, 8))
        u.append(("w3", i, 8))
    for j in range(8):
        u.append(("w2", (j, 0), 8))
        u.append(("w2", (j, 8), 8))
        u.append(("w2", (j, 16), 6))
    return u


UNITS = unit_list()
NU = len(UNITS)


def host_wstream(inp, l):
    w_in = inp["w_in"][l]
    out = np.zeros((NU, 128, 8, 128), np.float32)

    def kmajor(mat):
        nk = mat.shape[0] // 128
        return mat.reshape(nk, 128, 128).transpose(1, 0, 2)

    for ui, (kind, arg, nk) in enumerate(UNITS):
        if kind == "in":
            out[ui] = kmajor(w_in[:, arg:arg + 128])
        elif kind == "small":
            blk = np.zeros((1024, 128), np.float32)
            blk[:, 0:16] = w_in[:, O_CG:O_CG + 16]
            blk[:, 16:20] = w_in[:, O_BETA:O_BETA + 4]
            blk[:, 20:24] = w_in[:, O_ALPHA:O_ALPHA + 4]
            out[ui] = kmajor(blk)
        elif kind == "awri":
            for wi, name in enumerate(("a_w_r", "a_w_i")):
                w = inp[name][l]
                for b in range(4):
                    m = np.zeros((128, 128), np.float32)
                    m[0:64, 0:64] = w[2 * b]
                    m[64:128, 64:128] = w[2 * b + 1]
                    out[ui, :, wi * 4 + b, :] = m
        elif kind == "cwg":
            w = inp["c_w_g2"][l]
            out[ui, 0:16, 0, :] = w[:, 0:128]
            out[ui, 0:16, 1, :] = w[:, 128:256]
        elif kind == "br01":
            j = arg
            wb = inp["w_branch"][l]
            out[ui, :, 0:4, :] = kmajor(wb[0][:, 128 * j:128 * (j + 1)])
            out[ui, :, 4:8, :] = kmajor(wb[1][:, 128 * j:128 * (j + 1)])
        elif kind == "br2":
            j = arg
            wb = inp["w_branch"][l]
            out[ui, :, 0:4, :] = kmajor(wb[2][:, 128 * j:128 * (j + 1)])
        elif kind == "wout":
            out[ui] = kmajor(inp["w_out"][l][:, 128 * arg:128 * (arg + 1)])
        elif kind == "w1":
            out[ui] = kmajor(inp["ffn_w1"][l][:, 128 * arg:128 * (arg + 1)])
        elif kind == "w3":
            out[ui] = kmajor(inp["ffn_w3"][l][:, 128 * arg:128 * (arg + 1)])
        elif kind == "w2":
            j, k0 = arg
            out[ui, :, 0:nk, :] = kmajor(inp["ffn_w2"][l][128 * k0:128 * (k0 + nk), 128 * j:128 * (j + 1)])
    return out


def host_consts():
    j = np.arange(128)[:, None]
    i = np.arange(128)[None, :]
    cst = np.zeros((128, 6, 128), np.float32)
    cst[:, 0] = (i == j)
    cst[:, 1] = (i >= j)
    cst[:, 2] = np.where(i < j, -1.0e4, 0.0)
    cst[:, 3] = (i > j)
    cst[:, 4] = 1.0
    cst[:, 5] = 1.0 / 1024.0
    esel = np.zeros((24, 4, 128), np.float32)
    for h in range(4):
        esel[16 + h, h, :] = 1.0
    return cst, esel


def host_params(inp, depth):
    pp = np.zeros((128, depth, NPP), np.float32)

    def fm(v):
        return v.reshape(-1, 128).T

    for l in range(depth):
        pp[:, l, PP_ACW:PP_ACW + 16] = inp["a_conv_w"][l].reshape(4, 4, 128).transpose(2, 1, 0).reshape(128, 16)
        pp[:, l, PP_ACB:PP_ACB + 4] = fm(inp["a_conv_b"][l])
        pp[:, l, PP_ABR:PP_ABR + 4] = fm(inp["a_b_r"][l])
        pp[:, l, PP_ABI:PP_ABI + 4] = fm(inp["a_b_i"][l])
        pp[:, l, PP_ALAM:PP_ALAM + 4] = fm(inp["a_lambda"][l])
        pp[:, l, PP_BCW:PP_BCW + 48] = inp["b_conv_w"][l].reshape(4, 12, 128).transpose(2, 1, 0).reshape(128, 48)
        pp[:, l, PP_BNW] = inp["b_norm_w"][l]
        pp[:, l, PP_CNW] = inp["c_norm_w"][l]
        pp[:, l, PP_CBG:PP_CBG + 2] = fm(inp["c_b_g2"][l])
        pp[:, l, PP_GB:PP_GB + 24] = fm(inp["gate_b"][l])
        pp[:, l, PP_LN1G:PP_LN1G + 8] = fm(inp["ln1_g"][l])
        pp[:, l, PP_LN1B:PP_LN1B + 8] = fm(inp["ln1_b"][l])
        pp[:, l, PP_LN2G:PP_LN2G + 8] = fm(inp["ln2_g"][l])
        pp[:, l, PP_LN2B:PP_LN2B + 8] = fm(inp["ln2_b"][l])
    pt = np.zeros((1, depth * 8), np.float32)
    for l in range(depth):
        pt[0, l * 8:l * 8 + 4] = inp["b_a_log"][l]
        pt[0, l * 8 + 4:l * 8 + 8] = inp["b_dt_bias"][l]
    return pp, pt


class _Stop(Exception):
    pass


def build_nc(seq=SEQ, depth=DEPTH, stop=None):
    NG = seq // T
    alpha2 = 2.0 * (2.0 * depth) ** 0.25
    nc = bass.Bass("TRN2", target_bir_lowering=False)
    xT = nc.dram_tensor("xT", [KC, 128, seq], F32, kind="ExternalInput").ap()
    wst = nc.dram_tensor("wst", [depth, NU, 128, 8, 128], F32, kind="ExternalInput").ap()
    cstd = nc.dram_tensor("cst", [128, 6, 128], F32, kind="ExternalInput").ap()
    eseld = nc.dram_tensor("esel", [24, 4, 128], F32, kind="ExternalInput").ap()
    ppd = nc.dram_tensor("pp", [128, depth, NPP], F32, kind="ExternalInput").ap()
    ptd = nc.dram_tensor("pt", [1, depth * 8], F32, kind="ExternalInput").ap()
    outd = nc.dram_tensor("out", [KC, 128, seq], F32, kind="ExternalOutput").ap()
    dbg_out = {}

    with ExitStack() as es:
        def sb(name, shape, dt=F32):
            return es.enter_context(nc.sbuf_tensor(name, shape, dt))

        def psum(name, shape, dt=F32):
            return es.enter_context(nc.psum_tensor(name, shape, dt))

        S = Sched(nc)
        xres = sb("xres", [128, KC, T])
        xb = sb("xb", [128, KC, T], BF16)
        wring = sb("wring", [128, NSLOT, 8, 128], BF16)
        cst = sb("cstf", [128, 6, 128])
        identf, Uf, NEGM, Lstrict, onesf, ones1k = (cst[:, k, :] for k in range(6))
        identb = sb("identb", [128, 128], BF16)
        onesb = sb("onesb", [128, 128], BF16)
        esel = sb("eself", [24, 4, 128])
        pp = sb("ppf", [128, depth, NPP])
        pder = sb("pder", [128, depth, 48])
        ptb = sb("ptb", [128, depth * 8])
        nexpal = sb("nexpal", [128, depth * 8])
        cb = sb("cbias", [128, 4])
        hst = sb("hst", [128, depth, 4])
        tailA = sb("tailA", [128, depth, 4, 3])
        tailB = sb("tailB", [128, depth, 12, 3])
        SB = sb("SBst", [128, depth, 4, 128])
        SBb = sb("SBstb", [128, depth, 4, 128], BF16)
        SC = sb("SCst", [128, depth, 2, 128])
        SCb = sb("SCstb", [128, depth, 2, 128], BF16)
        NP1 = 3
        P1 = sb("P1", [128, NP1, T + 3])
        NTMP = 8
        tmp = sb("tmp", [128, NTMP, T])
        awrib = sb("awrib", [128, 8, 128], BF16)
        cwgb = sb("cwgb", [128, 2, 128], BF16)
        sqb2 = sb("sqb2", [128, 2, T], BF16)
        t44 = sb("t44", [128, NCH, 4]); t44b = sb("t44b", [128, NCH, 4])
        xa = sb("xa", [128, T])
        xab = sb("xab", [128, T], BF16)
        qn = sb("qn", [128, 4, T], BF16)
        kn = sb("kn", [128, 4, T], BF16)
        vT2 = sb("vT2", [128, 4, T], BF16)
        betabc = sb("betabc", [128, 4, T], BF16)
        smallfm = sb("smallfm", [24, T])
        pcgb = sb("pcgb", [16, T], BF16)
        tok8 = sb("tok8", [128, NCH, 8])
        hbeta = sb("hbeta", [128, NCH, 4])
        beta = sb("beta", [128, NCH, 4])
        gtok = sb("gtok", [128, NCH, 4])
        lC = sb("lC", [128, 2, T])
        qC = sb("qC", [128, 2, T])
        kC = sb("kC", [128, 2, T])
        vCT = sb("vCT", [128, 4, T], BF16)
        gr = sb("gr", [128, 4, T], BF16)
        scr = sb("scr", [128, 12288], BF16)
        ya = scr[:, 0:2048].rearrange("p (b t) -> p b t", b=4)
        yb = scr[:, 2048:4096].rearrange("p (b t) -> p b t", b=4)
        yc = scr[:, 4096:6144].rearrange("p (b t) -> p b t", b=4)
        merged = scr[:, 6144:10240].rearrange("p (b t) -> p b t", b=8)
        gz = scr[:, 10240:12288].rearrange("p (b t) -> p b t", b=4)
        hact = scr[:, 0:NFB * T].rearrange("p (b t) -> p b t", b=NFB)
        for b in range(4):
            S.region("ya%d" % b, "scr", b * T, (b + 1) * T)
            S.region("yb%d" % b, "scr", 2048 + b * T, 2048 + (b + 1) * T)
            S.region("yc%d" % b, "scr", 4096 + b * T, 4096 + (b + 1) * T)
            S.region("gz%d" % b, "scr", 10240 + b * T, 10240 + (b + 1) * T)
        for b in range(8):
            S.region("mg%d" % b, "scr", 6144 + b * T, 6144 + (b + 1) * T)
        for b in range(NFB):
            S.region("h%d" % b, "scr", b * T, (b + 1) * T)
        gc4 = sb("gc4", [128, 4]); ngc4 = sb("ngc4", [128, 4]); eg4 = sb("eg4", [128, 4])
        egl4 = sb("egl4", [128, 4]); dl4 = sb("dl4", [128, 4]); bge4 = sb("bge4", [128, 4])
        t4 = sb("t4", [128, 4])
        gbc = sb("gbc", [128, 4, 128])
        egbc = sb("egbc", [128, 4, 128])
        Dm = sb("Dm", [128, 4, 128])
        Dsb = sb("Dsb", [128, 4, 128])
        vb_t = sb("vb_t", [128, 4, 128], BF16); kbg_t = sb("kbg_t", [128, 4, 128], BF16); kd_t = sb("kd_t", [128, 4, 128], BF16)
        nAT = sb("nAT", [128, 4, 128]); nA = sb("nA", [128, 4, 128]); TTb = sb("TTb", [128, 4, 128], BF16); attnT = sb("attnT", [128, 4, 128], BF16)
        QP = sb("QP", [128, 4, 2, 256]); PB = sb("PBm", [128, 4, 2, 128])
        nwT = sb("nwT", [128, 4, 128], BF16); vnew = sb("vnew", [128, 4, 128], BF16); qgT = sb("qgT", [128, 4, 128], BF16)
        on_t = sb("on_t", [128, 8, 128], BF16); junk = sb("junk", [128, 8, 128], BF16)
        ssq1 = sb("ssq1", [128, 8]); ln1 = sb("ln1", [128, 8]); rs1 = sb("rs1", [128, 8])
        clC = sb("clC", [128, 2, 128]); ebC = sb("ebC", [128, 2, 128]); enbC = sb("enbC", [128, 2, 128]); edecC = sb("edecC", [128, 2, 128])
        nclC = sb("nclC", [128, 2]); dlcC = sb("dlcC", [128, 2])
        qin = sb("qin", [128, 2, 128], BF16); kin = sb("kin", [128, 2, 128], BF16); kdecT = sb("kdecT", [128, 2, 128], BF16)
        kdtok = sb("kdtok", [128, 2, 128], BF16); vtokC = sb("vtokC", [128, 4, 128], BF16); attnC = sb("attnC", [128, 4, 128], BF16)
        msq = gbc[:].rearrange("p h i -> p (h i)")
        rstd = egbc[:].rearrange("p h i -> p (h i)")
        meansb = Dm[:].rearrange("p h i -> p (h i)")

        pf = [psum("pf%d" % i, [128, 512]) for i in range(8)]
        PFN = ["pf%d" % i for i in range(8)]

        sems = {e: es.enter_context(nc.semaphore("s_" + e)) for e in Sched.ENGS}
        dnames = ["w%d" % i for i in range(NSLOT)] + ["ld", "st", "c0", "c1", "c2", "c3"]
        dsems = {k: es.enter_context(nc.semaphore("d_" + k)) for k in dnames}

        CBI = {1.0: 0, 1e-6: 1, 4e-5: 2, 0.0: 3}

        def ACT(out, in_, func, r, w, scale=None, bias=None, accum=None):
            kw = {}
            if scale is not None: kw["scale"] = scale
            if bias is None:
                bias = 0.0
            if isinstance(bias, float):
                k_ = CBI[bias]
                bias = cb[0:in_.shape[0], k_:k_ + 1]
                r = list(r) + ["cb"]
            kw["bias"] = bias
            if accum is not None: kw["accum_out"] = accum
            S.op("act", lambda e: e.activation(out=out, in_=in_, func=func, **kw), reads=r, writes=w)

        def TS(out, in0, s1, s2, op0, op1, r, w, eng="dve"):
            if op1 is None:
                S.op(eng, lambda e: e.tensor_scalar(out=out, in0=in0, scalar1=s1, scalar2=None, op0=op0), reads=r, writes=w)
            else:
                S.op(eng, lambda e: e.tensor_scalar(out=out, in0=in0, scalar1=s1, scalar2=s2, op0=op0, op1=op1), reads=r, writes=w)

        def STT(out, in0, sc, in1, op0, op1, r, w, eng="dve"):
            S.op(eng, lambda e: e.scalar_tensor_tensor(out=out, in0=in0, scalar=sc, in1=in1, op0=op0, op1=op1), reads=r, writes=w)

        def TT(out, in0, in1, op, r, w, eng="dve"):
            S.op(eng, lambda e: e.tensor_tensor(out=out, in0=in0, in1=in1, op=op), reads=r, writes=w)

        def CP(out, in_, r, w, eng="dve"):
            if eng == "act":
                ACT(out, in_, AF.Identity, r, w)
            else:
                S.op(eng, lambda e: e.tensor_copy(out=out, in_=in_), reads=r, writes=w)

        def MM(out, lhsT, rhs, start, stop, r, w):
            S.op("pe", lambda e: e.matmul(out, lhsT=lhsT, rhs=rhs, start=start, stop=stop), reads=r, writes=w)

        def TR(out, in_, ident, r, w):
            S.op("pe", lambda e: e.transpose(out, in_, ident), reads=r, writes=w)

        glist = []
        for g in range(NG):
            for l in range(depth):
                for ui, (kind, arg, nk) in enumerate(UNITS):
                    glist.append((l, ui, nk))
        wstate = {"issued": 0, "next": 0}

        def w_issue():
            n = wstate["issued"]
            if n >= len(glist):
                return
            l, ui, nk = glist[n]
            slot = n % NSLOT
            S.op("pool", lambda e: e.dma_start(out=wring[:, slot, 0:nk, :], in_=wst[l, ui, :, 0:nk, :]),
                 writes=["ws%d" % slot], dma="w%d" % slot)
            wstate["issued"] += 1

        def w_get(kind):
            n = wstate["next"]
            l, ui, nk = glist[n]
            assert UNITS[ui][0] == kind, (UNITS[ui], kind)
            slot = n % NSLOT
            wstate["next"] += 1
            return wring[:, slot], "ws%d" % slot

        def w_done():
            w_issue()

        for _ in range(NSLOT):
            w_issue()
        S.op("sp", lambda e: e.dma_start(out=cst[:], in_=cstd), writes=["cst"], dma="c0")
        S.op("sp", lambda e: e.dma_start(out=esel[:], in_=eseld), writes=["esel"], dma="c1")
        S.op("sp", lambda e: e.dma_start(out=pp[:], in_=ppd), writes=["pp"], dma="c2")
        S.op("sp", lambda e: e.dma_start(out=ptb[:], in_=ptd.partition_broadcast(128)), writes=["ptb"], dma="c3")
        for v_, k_ in CBI.items():
            S.op("dve", lambda e, v_=v_, k_=k_: e.memset(cb[:, k_:k_ + 1], v_), writes=["cb"])
        CP(identb[:], identf, ["cst"], ["identb"])
        CP(onesb[:], onesf, ["cst"], ["onesb"])
        S.op("dve", lambda e: e.memset(hst[:], 0.0), writes=["hst"])
        S.op("dve", lambda e: e.memset(tailA[:], 0.0), writes=["tailA"])
        S.op("dve", lambda e: e.memset(tailB[:], 0.0), writes=["tailB"])
        S.op("dve", lambda e: e.memset(SB[:], 0.0), writes=["SB"])
        S.op("dve", lambda e: e.memset(SBb[:], 0.0), writes=["SBb"])
        S.op("dve", lambda e: e.memset(SC[:], 0.0), writes=["SC"])
        S.op("dve", lambda e: e.memset(SCb[:], 0.0), writes=["SCb"])
        for l in range(depth):
            TS(pder[:, l, 0:8], pp[:, l, PP_ABR:PP_ABR + 8], 0.5, None, ALU.mult, None, ["pp"], ["pder"])
            ACT(pder[:, l, 8:12], pp[:, l, PP_ALAM:PP_ALAM + 4], AF.Exp, ["pp", "pder"], ["pder"], scale=-1.0)
            ACT(pder[:, l, 8:12], pder[:, l, 8:12], AF.Ln, ["pder"], ["pder"], bias=1.0)
            TS(pder[:, l, 12:16], pder[:, l, 8:12], -8.0, None, ALU.mult, None, ["pder"], ["pder"])
            TS(pder[:, l, 8:12], pder[:, l, 8:12], -4.0, None, ALU.mult, None, ["pder"], ["pder"])
            TS(pder[:, l, 16:18], pp[:, l, PP_BNW:PP_BNW + 2], 0.5, None, ALU.mult, None, ["pp", "pder"], ["pder"])
            TS(pder[:, l, 18:20], pp[:, l, PP_CBG:PP_CBG + 2], -1.0, None, ALU.mult, None, ["pp", "pder"], ["pder"])
            TS(pder[:, l, 20:44], pp[:, l, PP_GB:PP_GB + 24], 0.5, None, ALU.mult, None, ["pp", "pder"], ["pder"])
        ACT(nexpal[:], ptb[:], AF.Exp, ["ptb"], ["nexpal"])
        TS(nexpal[:], nexpal[:], -1.0, None, ALU.mult, None, ["nexpal"], ["nexpal"])

        bank_rr = {"i": 0}

        def newbank():
            i = (0, 1, 2, 3, 5, 6)[bank_rr["i"] % 6]
            bank_rr["i"] += 1
            return pf[i], PFN[i]

        tmp_rr = {"i": 0}

        def newtmp():
            i = tmp_rr["i"] % NTMP
            tmp_rr["i"] += 1
            return tmp[:, i, :], "tmp%d" % i

        p1_rr = {"i": 0}

        def newp1():
            i = p1_rr["i"] % NP1
            p1_rr["i"] += 1
            return P1[:, i, :], "P1_%d" % i

        def proj(kind="in"):
            wv, wn = w_get(kind)
            bk, bn = newbank()
            for k in range(KC):
                MM(bk[:], wv[:, k, :], xb[:, k, :], k == 0, k == KC - 1, [wn, "xb"], [bn])
            w_done()
            return bk, bn

        def run_all():
          for g in range(NG):
              tsl = slice(g * T, (g + 1) * T)
              S.op("sp", lambda e, tsl=tsl: e.dma_start(out=xres[:], in_=xT[:, :, tsl].rearrange("k p t -> p k t")),
                   writes=["xres"], dma="ld")
              for k in range(KC):
                  CP(xb[:, k, :], xres[:, k, :], ["xres"], ["xb"], eng=("act" if k % 2 else "dve"))
              for l in range(depth):
                  pd = pder[:, l, :]
                  ppl = pp[:, l, :]

                  awrib_n = "awrib"
                  for b in range(4):
                      bx, bxn = proj()
                      if b == 0:
                          wv, wn = w_get("awri")
                          CP(awrib[:], wv[:], [wn], [awrib_n], eng="act")
                          w_done()
                      p1, p1n = newp1()
                      CP(p1[:, 0:3], tailA[:, l, b, :], ["tailA"], [p1n])
                      CP(p1[:, 3:T + 3], bx[:], [bxn], [p1n], eng="act")
                      cw = ppl[:, PP_ACW + 4 * b:PP_ACW + 4 * b + 4]
                      TS(xa[:], p1[:, 3:T + 3], cw[:, 3:4], ppl[:, PP_ACB + b:PP_ACB + b + 1], ALU.mult, ALU.add, [p1n, "pp"], ["xa"])
                      for kk in range(3):
                          STT(xa[:], p1[:, kk:kk + T], cw[:, kk:kk + 1], xa[:], ALU.mult, ALU.add, [p1n, "pp", "xa"], ["xa"])
                      CP(tailA[:, l, b, :], p1[:, T:T + 3], [p1n], ["tailA"])
                      CP(xab[:], xa[:], ["xa"], ["xab"], eng="act")
                      zr, zrn = newbank()
                      MM(zr[:], awrib[:, b, :], xab[:], True, True, [awrib_n, "xab"], [zrn])
                      zi, zin = newbank()
                      MM(zi[:], awrib[:, 4 + b, :], xab[:], True, True, [awrib_n, "xab"], [zin])
                      tr_, trn = newtmp()
                      ACT(tr_, zr[:], AF.Tanh, [zrn, "pder"], [trn], scale=0.5, bias=pd[:, b:b + 1])
                      ti_, tin = newtmp()
                      ACT(ti_, zi[:], AF.Tanh, [zin, "pder"], [tin], scale=0.5, bias=pd[:, 4 + b:5 + b])
                      a_, an = newtmp()
                      TS(a_, tr_, pd[:, 8 + b:9 + b], pd[:, 8 + b:9 + b], ALU.mult, ALU.add, [trn, "pder"], [an])
                      e2_, e2n = newtmp()
                      ACT(e2_, a_, AF.Exp, [an], [e2n], scale=2.0)
                      ACT(a_, a_, AF.Exp, [an], [an])
                      TS(e2_, e2_, -1.0, 1.0, ALU.mult, ALU.add, [e2n], [e2n])
                      ACT(e2_, e2_, AF.Ln, [e2n], [e2n])
                      ACT(e2_, e2_, AF.Exp, [e2n], [e2n], scale=0.5)
                      STT(ti_, ti_, 1.0, xa[:], ALU.add, ALU.mult, [tin, "xa"], [tin])
                      TT(ti_, ti_, e2_, ALU.mult, [tin, e2n], [tin])
                      S.op("dve", lambda e, o=tr_, a_=a_, u=ti_, l=l, b=b: e.tensor_tensor_scan(
                          out=o, data0=a_, data1=u, initial=hst[:, l, b:b + 1], op0=ALU.mult, op1=ALU.add),
                          reads=[an, tin, "hst"], writes=[trn])
                      CP(hst[:, l, b:b + 1], tr_[:, T - 1:T], [trn], ["hst"])
                      bg, bgn = proj()
                      g2, g2n = e2_, e2n
                      ACT(g2, bg[:], AF.Square, [bgn], [g2n])
                      TS(g2, g2, 0.044715, 1.0, ALU.mult, ALU.add, [g2n], [g2n])
                      TT(g2, g2, bg[:], ALU.mult, [g2n, bgn], [g2n])
                      ACT(g2, g2, AF.Tanh, [g2n], [g2n], scale=0.7978845608028654)
                      STT(g2, g2, 1.0, bg[:], ALU.add, ALU.mult, [g2n, bgn], [g2n])
                      STT(ya[:, b, :], tr_, 0.25, g2, ALU.mult, ALU.mult, [trn, g2n], ["ya%d" % b])

                  if stop == "A":
                      raise _Stop()
                  wv, wn = w_get("small")
                  bk, bn = newbank()
                  for k in range(KC):
                      MM(bk[0:24, :], wv[:, k, 0:24], xb[:, k, :], k == 0, k == KC - 1, [wn, "xb"], [bn])
                  for c in range(NCH):
                      for k in range(KC):
                          MM(pf[4][:, c * 8:(c + 1) * 8], xb[:, k, c * 128:(c + 1) * 128], wv[:, k, 16:24], k == 0, k == KC - 1,
                             [wn, "xb"], [PFN[4]])
                  w_done()
                  CP(smallfm[:], bk[0:24, :], [bn], ["smallfm"], eng="act")
                  CP(pcgb[:], bk[0:16, :], [bn], ["pcgb"])
                  CP(tok8[:], pf[4][:, 0:32].rearrange("p (c e) -> p c e", c=NCH), [PFN[4]], ["tok8"], eng="act")
                  ACT(t44[:], tok8[:, :, 0:4], AF.Tanh, ["tok8"], ["t44"], scale=0.5)
                  TS(beta[:], t44[:], 0.5, 0.5, ALU.mult, ALU.add, ["t44"], ["beta"])
                  TS(hbeta[:], t44[:], 0.25, 0.25, ALU.mult, ALU.add, ["t44"], ["hbeta"])
                  TT(t44b[:], tok8[:, :, 4:8], ptb[:, l * 8 + 4:l * 8 + 8].unsqueeze(1).to_broadcast([128, NCH, 4]), ALU.add,
                     ["tok8", "ptb"], ["t44b"])
                  ACT(t44b[:], t44b[:], AF.Exp, ["t44b"], ["t44b"])
                  ACT(t44b[:], t44b[:], AF.Ln, ["t44b"], ["t44b"], bias=1.0)
                  TT(gtok[:], t44b[:], nexpal[:, l * 8:l * 8 + 4].unsqueeze(1).to_broadcast([128, NCH, 4]), ALU.mult,
                     ["t44b", "nexpal"], ["gtok"])
                  for h in range(4):
                      bb, bbn = newbank()
                      MM(bb[:], esel[:, h, :], smallfm[:], True, True, ["esel", "smallfm"], [bbn])
                      t_, tn = newtmp()
                      ACT(t_, bb[:], AF.Tanh, [bbn], [tn], scale=0.5)
                      TS(betabc[:, h, :], t_, 0.5, 0.5, ALU.mult, ALU.add, [tn], ["betabc%d" % h])
                  wv, wn = w_get("cwg")
                  CP(cwgb[:], wv[:, 0:2, :], [wn], ["cwgb"], eng="act")
                  w_done()
                  for b in range(2):
                      bb, bbn = newbank()
                      MM(bb[:], cwgb[0:16, b, :], pcgb[:], True, True, ["cwgb", "pcgb"], [bbn])
                      t_, tn = newtmp()
                      ACT(t_, bb[:], AF.Exp, [bbn, "pder"], [tn], scale=-1.0, bias=pd[:, 18 + b:19 + b])
                      ACT(lC[:, b, :], t_, AF.Ln, [tn], ["lC%d" % b], bias=1.0)

                  if stop == "small":
                      raise _Stop()
                  def run_pipe(gen_list, W=2):
                      active = []
                      it = iter(gen_list)
                      while True:
                          while len(active) < W:
                              try:
                                  active.append(next(it))
                              except StopIteration:
                                  break
                          if not active:
                              break
                          for g_ in list(active):
                              try:
                                  next(g_)
                              except StopIteration:
                                  active.remove(g_)

                  def gen_B(ci):
                      h = ci % 4
                      sq_ = sqb2[:, ci % 2, :]; sqn_ = "sqb%d" % (ci % 2)
                      bk, bn = proj()
                      p1, p1n = newp1()
                      CP(p1[:, 0:3], tailB[:, l, ci, :], ["tailB%d" % ci], [p1n])
                      CP(p1[:, 3:T + 3], bk[:], [bn], [p1n], eng="act")
                      yield
                      cw = ppl[:, PP_BCW + 4 * ci:PP_BCW + 4 * ci + 4]
                      c_, cn = newtmp()
                      TS(c_, p1[:, 3:T + 3], cw[:, 3:4], None, ALU.mult, None, [p1n, "pp"], [cn])
                      for kk in range(3):
                          STT(c_, p1[:, kk:kk + T], cw[:, kk:kk + 1], c_, ALU.mult, ALU.add, [p1n, "pp", cn], [cn])
                      CP(tailB[:, l, ci, :], p1[:, T:T + 3], [p1n], ["tailB%d" % ci])
                      yield
                      t_, tn = newtmp()
                      ACT(t_, c_, AF.Tanh, [cn], [tn], scale=0.5)
                      yield
                      if ci >= 8:
                          STT(vT2[:, h, :], t_, 1.0, c_, ALU.add, ALU.mult, [tn, cn], ["vT2_%d" % h])
                          return
                      STT(c_, t_, 1.0, c_, ALU.add, ALU.mult, [tn, cn], [cn])
                      yield
                      ACT(sq_, c_, AF.Square, [cn], [sqn_])
                      yield
                      b2, b2n = newbank()
                      MM(b2[:], onesb[:], sq_, True, True, ["onesb", sqn_], [b2n])
                      yield
                      ACT(t_, b2[:], AF.Ln, [b2n], [tn], scale=0.25, bias=1e-6)
                      ACT(t_, t_, AF.Exp, [tn], [tn], scale=-0.5)
                      yield
                      if ci < 4:
                          STT(qn[:, h, :], c_, 0.5 * (128.0 ** -0.5), t_, ALU.mult, ALU.mult, [cn, tn], ["qn%d" % h])
                      else:
                          STT(kn[:, h, :], c_, 0.5, t_, ALU.mult, ALU.mult, [cn, tn], ["kn%d" % h])

                  run_pipe([gen_B(ci) for ci in range(12)], W=2)
                  def gen_gate(h, dst, dn, col):
                      bk, bn = proj()
                      t_, tn = newtmp()
                      yield
                      ACT(t_, bk[:], AF.Tanh, [bn], [tn], scale=0.5)
                      yield
                      STT(t_, t_, 1.0, bk[:], ALU.add, ALU.mult, [tn, bn], [tn])
                      TS(dst[:, h, :], t_, pd[:, col:col + 1], None, ALU.mult, None, [tn, "pder"], [dn % h])

                  run_pipe([gen_gate(h, gz, "gz%d", 16) for h in range(4)], W=2)
                  if stop == "B":
                      raise _Stop()
                  for b in range(2):
                      bk, bn = proj()
                      CP(qC[:, b, :], bk[:], [bn], ["qC%d" % b], eng="act")
                  for b in range(2):
                      bk, bn = proj()
                      CP(kC[:, b, :], bk[:], [bn], ["kC%d" % b], eng="act")
                  for h in range(4):
                      bk, bn = proj()
                      CP(vCT[:, h, :], bk[:], [bn], ["vCT%d" % h], eng="act")
                  run_pipe([gen_gate(h, gr, "gr%d", 17) for h in range(4)], W=2)

                  if stop == "C":
                      raise _Stop()
                  pbv = pf[7][:].bitcast(BF16)
                  PB7 = PFN[7]

                  def epilogue_g(u, o_ap, o_bn, dst, gate, rn, wn_, trcol, pbv=None, PB7=None):
                      if pbv is None:
                          pbv = pf[7][:].bitcast(BF16); PB7 = PFN[7]
                      U_ = "%d" % u
                      ACT(junk[:, u, :], o_ap, AF.Square, [o_bn], ["junk" + U_, "ssq" + U_], accum=ssq1[:, u:u + 1])
                      yield
                      ACT(ln1[:, u:u + 1], ssq1[:, u:u + 1], AF.Ln, ["ssq" + U_], ["ln" + U_], scale=1.0 / 128.0, bias=1e-6)
                      ACT(rs1[:, u:u + 1], ln1[:, u:u + 1], AF.Exp, ["ln" + U_], ["rs" + U_], scale=-0.5)
                      yield
                      TS(on_t[:, u, :], o_ap, rs1[:, u:u + 1], None, ALU.mult, None, [o_bn, "rs" + U_], ["on" + U_])
                      yield
                      TR(pbv[:, trcol:trcol + 128], on_t[:, u, :], identb[:], ["on" + U_, "identb"], [PB7])
                      yield
                      TT(dst, pbv[:, trcol:trcol + 128], gate, ALU.mult, [PB7, rn], [wn_])

                  def gdn_head(c, csl, h):
                      bank = pf[h]; bn = PFN[h]
                      knc = kn[:, h, csl]; qnc = qn[:, h, csl]
                      H = "%d" % h
                      tc0 = h * 256
                      sbh = SBb[:, l, h, :]; sbn = "SBb%d_%d" % (l, h); sfn = "SB%d_%d" % (l, h)
                      TR(pbv[:, tc0:tc0 + 128], knc, identb[:], ["kn" + H, "identb"], [PB7])
                      TR(pbv[:, tc0 + 128:tc0 + 256], vT2[:, h, csl], identb[:], ["vT2_" + H, "identb"], [PB7])
                      MM(bank[:, 0:128], knc, knc, True, True, ["kn" + H], [bn])
                      MM(bank[:, 128:256], knc, qnc, True, True, ["kn" + H, "qn" + H], [bn])
                      yield
                      TS(vb_t[:, h, :], pbv[:, tc0 + 128:tc0 + 256], hbeta[:, c, h:h + 1], None, ALU.mult, None, [PB7, "hbeta"], ["vb" + H])
                      TS(kbg_t[:, h, :], pbv[:, tc0:tc0 + 128], bge4[:, h:h + 1], None, ALU.mult, None, [PB7, "bge4"], ["kbg" + H])
                      TS(kd_t[:, h, :], pbv[:, tc0:tc0 + 128], egl4[:, h:h + 1], None, ALU.mult, None, [PB7, "egl4"], ["kd" + H])
                      STT(nAT[:, h, :], bank[:, 0:128], -1.0, Dsb[:, h, :], ALU.mult, ALU.mult, [bn, "Dsb"], ["nAT" + H])
                      TT(attnT[:, h, :], bank[:, 128:256], Dm[:, h, :], ALU.mult, [bn, "Dm"], ["attnT" + H])
                      TT(qgT[:, h, :], qnc, egbc[:, h, :], ALU.mult, ["qn" + H, "egbc"], ["qgT" + H])
                      yield
                      TR(bank[:, 256:384], nAT[:, h, :], identf, ["nAT" + H, "cst"], [bn])
                      yield
                      CP(nA[:, h, :], bank[:, 256:384], [bn], ["nA" + H], eng="act")
                      TT(QP[:, h, 1, 0:128], nAT[:, h, :], identf, ALU.add, ["nAT" + H, "cst"], ["QP1q" + H])
                      yield
                      MM(bank[:, 0:128], nA[:, h, :], nAT[:, h, :], True, True, ["nA" + H, "nAT" + H], [bn])
                      MM(bank[:, 128:256], nAT[:, h, :], nA[:, h, :], True, True, ["nA" + H, "nAT" + H], [bn])
                      yield
                      CP(QP[:, h, 1, 128:256], bank[:, 0:128], [bn], ["QP1p" + H], eng="act")
                      CP(PB[:, h, 1, :], bank[:, 128:256], [bn], ["PB1" + H], eng="act")
                      yield
                      for k in range(1, 7):
                          cur = k % 2; nxt = (k + 1) % 2
                          qc, pc_, pbc = "QP%dq%s" % (cur, H), "QP%dp%s" % (cur, H), "PB%d%s" % (cur, H)
                          qx, px, pbx = "QP%dq%s" % (nxt, H), "QP%dp%s" % (nxt, H), "PB%d%s" % (nxt, H)
                          if k < 6:
                              MM(bank[:, 0:256], PB[:, h, cur, :], QP[:, h, cur, :], True, True, [pbc, qc, pc_], [bn])
                              MM(bank[:, 256:384], QP[:, h, cur, 128:256], PB[:, h, cur, :], True, True, [pbc, pc_], [bn])
                              yield
                              TT(QP[:, h, nxt, 0:128], bank[:, 0:128], QP[:, h, cur, 0:128], ALU.add, [bn, qc], [qx])
                              CP(QP[:, h, nxt, 128:256], bank[:, 128:256], [bn], [px])
                              CP(PB[:, h, nxt, :], bank[:, 256:384], [bn], [pbx])
                              yield
                          else:
                              MM(bank[:, 0:128], PB[:, h, cur, :], QP[:, h, cur, 0:128], True, True, [pbc, qc], [bn])
                              yield
                              TT(TTb[:, h, :], bank[:, 0:128], QP[:, h, cur, 0:128], ALU.add, [bn, qc], ["TTb" + H])
                              yield
                      MM(bank[:, 0:128], kbg_t[:, h, :], TTb[:, h, :], True, True, ["kbg" + H, "TTb" + H], [bn])
                      yield
                      ACT(nwT[:, h, :], bank[:, 0:128], AF.Identity, [bn], ["nwT" + H], scale=-1.0)
                      yield
                      MM(bank[:, 128:256], TTb[:, h, :], vb_t[:, h, :], True, False, ["TTb" + H, "vb" + H], [bn])
                      MM(bank[:, 128:256], nwT[:, h, :], sbh, False, True, ["nwT" + H, sbn], [bn])
                      yield
                      CP(vnew[:, h, :], bank[:, 128:256], [bn], ["vnew" + H])
                      yield
                      MM(bank[:, 256:384], qgT[:, h, :], sbh, True, False, ["qgT" + H, sbn], [bn])
                      MM(bank[:, 256:384], attnT[:, h, :], vnew[:, h, :], False, True, ["attnT" + H, "vnew" + H], [bn])
                      MM(bank[:, 384:512], kd_t[:, h, :], vnew[:, h, :], True, True, ["kd" + H, "vnew" + H], [bn])
                      yield
                      STT(SB[:, l, h, :], SB[:, l, h, :], dl4[:, h:h + 1], bank[:, 384:512], ALU.mult, ALU.add, [sfn, "dl4", bn], [sfn])
                      yield
                      CP(sbh, SB[:, l, h, :], [sfn], [sbn], eng="act")
                      yield from epilogue_g(h, bank[:, 256:384], bn, yb[:, h, csl], gz[:, h, csl], "gz" + H, "yb" + H, tc0)

                  def gla_head(c, csl, h):
                      b = h // 2; po = 64 * (h % 2); psl = slice(po, po + 64)
                      bi = (4, 5, 6, 4)[h]; bank = pf[bi]; bn = PFN[bi]
                      H = "%d" % h
                      scn = "SC%d_%d" % (l, h); scbn = "SCb%d_%d" % (l, h)
                      bvw = bank[:].bitcast(BF16)
                      TR(bvw[:, 768:896], vCT[:, h, csl], identb[:], ["vCT" + H, "identb"], [bn])
                      MM(bank[:, 0:128], kin[psl, b, :], qin[psl, b, :], True, True, ["kin%d" % b, "qin%d" % b], [bn])
                      yield
                      CP(vtokC[:, h, :], bvw[:, 768:896], [bn], ["vtokC" + H], eng="act")
                      TT(attnC[:, h, :], bank[:, 0:128], Uf, ALU.mult, [bn, "cst"], ["attnC" + H])
                      yield
                      MM(bank[:, 128:256], attnC[:, h, :], vtokC[:, h, :], True, False, ["attnC" + H, "vtokC" + H], [bn])
                      MM(bank[:, 128:256], qin[psl, b, :], SCb[psl, l, b, :], False, True, ["qin%d" % b, scbn], [bn])
                      MM(bank[:, 256:384], kdtok[:, b, :], vtokC[:, h, :], True, True, ["kdtok%d" % b, "vtokC" + H], [bn])
                      yield
                      STT(SC[psl, l, b, :], SC[psl, l, b, :], dlcC[psl, b:b + 1], bank[psl, 256:384], ALU.mult, ALU.add,
                          [scn, "dlcC%d" % b, bn], [scn])
                      yield
                      CP(SCb[psl, l, b, :], SC[psl, l, b, :], [scn], [scbn], eng="act")
                      yield from epilogue_g(4 + h, bank[:, 128:256], bn, yc[:, h, csl], gr[:, h, csl], "gr" + H, "yc" + H, 768, bvw, bn)

                  for c in range(NCH):
                      csl = slice(c * 128, (c + 1) * 128)
                      g1 = pf[5][:].rearrange("p (h i) -> p h i", h=4)
                      g2_ = pf[6][:].rearrange("p (h i) -> p h i", h=4)
                      MM(pf[4][:, 0:4], Uf, gtok[:, c, :], True, True, ["cst", "gtok"], [PFN[4]])
                      CP(gc4[:], pf[4][:, 0:4], [PFN[4]], ["gc4"], eng="act")
                      TS(ngc4[:], gc4[:], -1.0, None, ALU.mult, None, ["gc4"], ["ngc4"])
                      ACT(eg4[:], gc4[:], AF.Exp, ["gc4"], ["eg4"])
                      for h in range(4):
                          TS(gbc[:, h, :], onesf, gtok[:, c, h:h + 1], None, ALU.mult, None, ["cst", "gtok"], ["gbc%d" % h])
                      for h in range(4):
                          MM(g1[:, h, :], gbc[:, h, :], Uf, True, True, ["gbc%d" % h, "cst"], [PFN[5]])
                      for h in range(4):
                          MM(g2_[:, h, :], gbc[:, h, :], Uf, True, False, ["gbc%d" % h, "cst"], [PFN[6]])
                          MM(g2_[:, h, :], identf, NEGM, False, True, ["cst"], [PFN[6]])
                      for b in range(2):
                          S.op("dve", lambda e, b=b, csl=csl: e.tensor_tensor_scan(
                              out=clC[:, b, :], data0=onesf, data1=lC[:, b, csl], initial=0.0, op0=ALU.mult, op1=ALU.add),
                              reads=["cst", "lC%d" % b], writes=["clC%d" % b])
                          cn_ = "clC%d" % b
                          ACT(ebC[:, b, :], clC[:, b, :], AF.Exp, [cn_], ["ebC%d" % b], scale=-1.0 / 16)
                          ACT(enbC[:, b, :], clC[:, b, :], AF.Exp, [cn_], ["enbC%d" % b], scale=1.0 / 16)
                          TS(nclC[:, b:b + 1], clC[:, b, 127:128], -1.0 / 16, None, ALU.mult, None, [cn_], ["nclC%d" % b])
                          ACT(edecC[:, b, :], clC[:, b, :], AF.Exp, [cn_, "nclC%d" % b], ["edecC%d" % b], scale=1.0 / 16, bias=nclC[:, b:b + 1])
                          ACT(dlcC[:, b:b + 1], nclC[:, b:b + 1], AF.Exp, ["nclC%d" % b], ["dlcC%d" % b])
                          STT(qin[:, b, :], qC[:, b, csl], 0.125, ebC[:, b, :], ALU.mult, ALU.mult, ["qC%d" % b, "ebC%d" % b], ["qin%d" % b])
                          TT(kin[:, b, :], kC[:, b, csl], enbC[:, b, :], ALU.mult, ["kC%d" % b, "enbC%d" % b], ["kin%d" % b])
                          TT(kdecT[:, b, :], kC[:, b, csl], edecC[:, b, :], ALU.mult, ["kC%d" % b, "edecC%d" % b], ["kdecT%d" % b])
                      ACT(egbc[:], g1, AF.Exp, [PFN[5]], ["egbc"])
                      for h in range(4):
                          ACT(Dm[:, h, :], g2_[:, h, :], AF.Exp, [PFN[6], "ngc4"], ["Dm"], bias=ngc4[:, h:h + 1])
                      for h in range(4):
                          ACT(egl4[:, h:h + 1], g1[:, h, 127:128], AF.Exp, [PFN[5], "ngc4"], ["egl4"], bias=ngc4[:, h:h + 1])
                      CP(dl4[:], egbc[:, :, 127], ["egbc"], ["dl4"])
                      TT(bge4[:], beta[:, c, :], eg4[:], ALU.mult, ["beta", "eg4"], ["bge4"])
                      TT(Dsb[:], Dm[:], Lstrict.unsqueeze(1).to_broadcast([128, 4, 128]), ALU.mult, ["Dm", "cst"], ["Dsb"])
                      TT(Dsb[:], Dsb[:], betabc[:, :, csl], ALU.mult, ["Dsb"] + ["betabc%d" % h for h in range(4)], ["Dsb"])
                      for b in range(2):
                          TR(pbv[:, b * 128:(b + 1) * 128], kdecT[:, b, :], identb[:], ["kdecT%d" % b, "identb"], [PB7])
                      for b in range(2):
                          CP(kdtok[:, b, :], pbv[:, b * 128:(b + 1) * 128], [PB7], ["kdtok%d" % b], eng="act")
                      def chain(*gs):
                          for g_ in gs:
                              yield from g_
                      gens = [gdn_head(c, csl, h) for h in range(4)] + [chain(gla_head(c, csl, 0), gla_head(c, csl, 3)),
                                                                         gla_head(c, csl, 1), gla_head(c, csl, 2)]
                      while gens:
                          for gen in list(gens):
                              try:
                                  next(gen)
                              except StopIteration:
                                  gens.remove(gen)

                  if stop == "mix" and l == depth - 1 and g == NG - 1:
                      raise _Stop()

                  for j in range(8):
                      tg = []
                      for gi in range(3):
                          bk, bn = proj()
                          t_, tn = newtmp()
                          ACT(t_, bk[:], AF.Tanh, [bn, "pder"], [tn], scale=0.5, bias=pd[:, 20 + gi * 8 + j:21 + gi * 8 + j])
                          tg.append((t_, tn))
                      wv, wn = w_get("br01")
                      b0, b0n = newbank()
                      for k in range(4):
                          MM(b0[:], wv[:, k, :], ya[:, k, :], k == 0, k == 3, [wn, "ya%d" % k], [b0n])
                      b1, b1n = newbank()
                      for k in range(4):
                          MM(b1[:], wv[:, 4 + k, :], yb[:, k, :], k == 0, k == 3, [wn, "yb%d" % k], [b1n])
                      w_done()
                      wv, wn = w_get("br2")
                      b2, b2n = newbank()
                      for k in range(4):
                          MM(b2[:], wv[:, k, :], yc[:, k, :], k == 0, k == 3, [wn, "yc%d" % k], [b2n])
                      w_done()
                      m_, mn = newtmp()
                      STT(m_, tg[0][0], 1.0, b0[:], ALU.add, ALU.mult, [tg[0][1], b0n], [mn])
                      STT(tg[1][0], tg[1][0], 1.0, b1[:], ALU.add, ALU.mult, [tg[1][1], b1n], [tg[1][1]])
                      TT(m_, m_, tg[1][0], ALU.add, [mn, tg[1][1]], [mn])
                      STT(tg[2][0], tg[2][0], 1.0, b2[:], ALU.add, ALU.mult, [tg[2][1], b2n], [tg[2][1]])
                      TT(merged[:, j, :], m_, tg[2][0], ALU.add, [mn, tg[2][1]], ["mg%d" % j])
                  for j in range(8):
                      wv, wn = w_get("wout")
                      bk, bn = newbank()
                      for k in range(KC):
                          MM(bk[:], wv[:, k, :], merged[:, k, :], k == 0, k == KC - 1, [wn, "mg%d" % k], [bn])
                      w_done()
                      STT(xres[:, j, :], xres[:, j, :], alpha2, bk[:], ALU.mult, ALU.add, ["xres", bn], ["xres"])

                  def layer_norm(gcol, bcol):
                      GBCN = ["gbc%d" % h_ for h_ in range(4)]
                      for k in range(KC):
                          MM(pf[4][:], ones1k, xres[:, k, :], k == 0, k == KC - 1, ["cst", "xres"], [PFN[4]])
                      be, ben = newbank()
                      for k in range(KC):
                          sq, sqn = newtmp()
                          ACT(sq, xres[:, k, :], AF.Square, ["xres"], [sqn])
                          MM(be[:], ones1k, sq, k == 0, k == KC - 1, ["cst", sqn], [ben])
                      CP(meansb, pf[4][:], [PFN[4]], ["Dm"], eng="act")
                      ACT(msq, pf[4][:], AF.Square, [PFN[4]], GBCN)
                      TT(rstd, be[:], msq, ALU.subtract, [ben, *GBCN], ["egbc"])
                      ACT(rstd, rstd, AF.Ln, ["egbc"], ["egbc"], bias=4e-5)
                      ACT(rstd, rstd, AF.Exp, ["egbc"], ["egbc"], scale=-0.5)
                      for k in range(KC):
                          t_, tn = newtmp()
                          TT(t_, xres[:, k, :], meansb, ALU.subtract, ["xres", "Dm"], [tn])
                          TT(t_, t_, rstd, ALU.mult, [tn, "egbc"], [tn])
                          ACT(xres[:, k, :], t_, AF.Identity, [tn, "pp"], ["xres"], scale=ppl[:, gcol + k:gcol + k + 1],
                              bias=ppl[:, bcol + k:bcol + k + 1])
                          ACT(xb[:, k, :], t_, AF.Identity, [tn, "pp"], ["xb"], scale=ppl[:, gcol + k:gcol + k + 1],
                              bias=ppl[:, bcol + k:bcol + k + 1])

                  layer_norm(PP_LN1G, PP_LN1B)
                  for i in range(NFB):
                      b1, b1n = proj("w1")
                      b3, b3n = proj("w3")
                      t_, tn = newtmp()
                      ACT(t_, b1[:], AF.Tanh, [b1n], [tn], scale=0.5)
                      STT(t_, t_, 1.0, b1[:], ALU.add, ALU.mult, [tn, b1n], [tn])
                      TT(hact[:, i, :], t_, b3[:], ALU.mult, [tn, b3n], ["h%d" % i])
                  for j in range(8):
                      bk, bn = newbank()
                      for (k0, nk) in ((0, 8), (8, 8), (16, 6)):
                          wv, wn = w_get("w2")
                          for k in range(nk):
                              MM(bk[:], wv[:, k, :], hact[:, k0 + k, :], (k0 + k) == 0, (k0 + k) == NFB - 1, [wn, "h%d" % (k0 + k)], [bn])
                          w_done()
                      STT(xres[:, j, :], xres[:, j, :], alpha2, bk[:], ALU.mult, ALU.add, ["xres", bn], ["xres"])
                  layer_norm(PP_LN2G, PP_LN2B)
              S.op("sp", lambda e, tsl=tsl: e.dma_start(out=outd[:, :, tsl].rearrange("k p t -> p k t"), in_=xres[:]),
                   reads=["xres"], dma="st")

        try:
            run_all()
        except _Stop:
            pass
        dbg_out = {}
        if stop == "mix":
            for nm, src in (("d_ya", ya), ("d_yb", yb), ("d_yc", yc)):
                dt_ = nc.dram_tensor(nm, [128, 4, T], BF16, kind="ExternalOutput").ap()
                S.op("sp", lambda e, dt_=dt_, src=src: e.dma_start(out=dt_, in_=src), reads=["ya%d" % b for b in range(4)] + ["yb%d" % b for b in range(4)] + ["yc%d" % b for b in range(4)], dma="st")
        S.emit(sems, dsems)
    return nc


_CACHE = {}


def host_inputs(inp, depth=DEPTH, seq=SEQ):
    cst, esel = host_consts()
    pp, pt = host_params(inp, depth)
    wst = np.stack([host_wstream(inp, l) for l in range(depth)], 0)
    shared = {"wst": wst, "cst": cst, "esel": esel, "pp": pp, "pt": pt}
    x = np.asarray(inp["x"], np.float32)
    maps = []
    for b in range(x.shape[0]):
        xT = np.ascontiguousarray(x[b].T).reshape(KC, 128, seq)
        m = dict(shared)
        m["xT"] = xT
        maps.append(m)
    return maps


def kernel(**inputs):
    inp = {k: np.asarray(v) for k, v in inputs.items()}
    B = inp["x"].shape[0]
    maps = host_inputs(inp)
    if "nc" not in _CACHE:
        _CACHE["nc"] = build_nc()
    nc = _CACHE["nc"]
    res = run_bass_kernel_spmd(nc, maps, core_ids=list(range(B)))
    out = np.empty((B, SEQ, D), np.float32)
    for b in range(B):
        o = np.asarray(res.results[b]["out"]).reshape(D, SEQ)
        out[b] = o.T
    return out
```
